# Optimizing a Trainium2 kernel written in Bass

```python
import math
import jax, jax.numpy as jnp
from jax import lax
import numpy as np

D_MODEL = 1024
BATCH = 16
SEQ = 2048
DEPTH = 2

N_MIXERS = 2
N_POOL_LAYERS = (DEPTH + 1) // 2
N_RG_LAYERS = DEPTH // 2
MEM_LEN = 256
POOL_WINDOWS = (2, 4, 8, 16)
POOL_GROUPS = len(POOL_WINDOWS)
POOL_GW = D_MODEL // POOL_GROUPS
D_RNN = D_MODEL
RG_HEADS = 4
RG_BLOCK = D_RNN // RG_HEADS
CONV_W = 4
RG_C = 8.0
XA_HEADS = 4
XA_HEAD_DIM = D_MODEL // XA_HEADS
N_GROUPS = 4
EXPERTS_PER_GROUP = 4
N_EXPERTS = N_GROUPS * EXPERTS_PER_GROUP
TOP_K_IN_GROUP = 2
D_EXPERT = D_MODEL // 2
RMS_EPS = 1e-6

kernel_name = "hybrid_pool_rglru_memxattn_hmoe"


def _rms_norm(x, g):
    xf = x.astype(jnp.float32)
    y = xf * lax.rsqrt(jnp.mean(xf * xf, axis=-1, keepdims=True) + RMS_EPS)
    return (y * g.astype(jnp.float32)).astype(x.dtype)


def _pool_mixer(xn, w_grp, scale):
    B, S, D = xn.shape
    xf = xn.astype(jnp.float32)
    pos = jnp.arange(1, S + 1, dtype=jnp.int32)
    outs = []
    for g, w in enumerate(POOL_WINDOWS):
        xg = xf[..., g * POOL_GW:(g + 1) * POOL_GW]
        c = jnp.cumsum(xg, axis=1)
        c_shift = jnp.pad(c, ((0, 0), (w, 0), (0, 0)))[:, :S]
        count = jnp.minimum(pos, w).astype(jnp.float32)[None, :, None]
        outs.append((c - c_shift) / count - xg)
    p = jnp.concatenate(outs, axis=-1).astype(xn.dtype).reshape(B, S, POOL_GROUPS, POOL_GW)
    y = jnp.einsum('bsgi,gio->bsgo', p, w_grp).reshape(B, S, D)
    return y * scale


def _causal_depthwise_conv(x, w, b):
    S = x.shape[1]
    xp = jnp.pad(x, ((0, 0), (CONV_W - 1, 0), (0, 0)))
    y = b
    for k in range(CONV_W):
        y = y + xp[:, k:k + S] * w[k]
    return y


def _lru_combine(c1, c2):
    a1, b1 = c1
    a2, b2 = c2
    return a1 * a2, a2 * b1 + b2


def _rglru_block(xn, w_in, conv_w, conv_b, w_gate, b_gate, lam, w_out):
    B, S, _ = xn.shape
    proj = xn @ w_in
    xb, yb = proj[..., :D_RNN], proj[..., D_RNN:]
    gate_branch = jax.nn.gelu(yb, approximate=True)
    xc = _causal_depthwise_conv(xb, conv_w, conv_b)
    xh = xc.reshape(B, S, RG_HEADS, RG_BLOCK)
    gates = jnp.einsum('bshi,hio->bsho', xh, w_gate) + b_gate
    r = jax.nn.sigmoid(gates[..., :RG_BLOCK].astype(jnp.float32)).reshape(B, S, D_RNN)
    i = jax.nn.sigmoid(gates[..., RG_BLOCK:].astype(jnp.float32)).reshape(B, S, D_RNN)
    log_a = -RG_C * r * jax.nn.softplus(-lam.astype(jnp.float32))
    a = jnp.exp(log_a)
    mult = jnp.sqrt(-jnp.expm1(2.0 * log_a))
    bterm = mult * (i * xc.astype(jnp.float32))
    _, h = lax.associative_scan(_lru_combine, (a, bterm), axis=1)
    y = h.astype(xn.dtype) * gate_branch
    return y @ w_out


def _mem_attention(xn, memn, wq, wkv, wo):
    B, S, D = xn.shape
    M = memn.shape[1]
    q = (xn @ wq).reshape(B, S, XA_HEADS, XA_HEAD_DIM)
    kv = memn @ wkv
    k = kv[..., :D].reshape(B, M, XA_HEADS, XA_HEAD_DIM)
    v = kv[..., D:].reshape(B, M, XA_HEADS, XA_HEAD_DIM)
    s = jnp.einsum('bshd,bmhd->bhsm', q, k).astype(jnp.float32) * (XA_HEAD_DIM ** -0.5)
    p = jax.nn.softmax(s, axis=-1).astype(v.dtype)
    o = jnp.einsum('bhsm,bmhd->bshd', p, v).reshape(B, S, D)
    return o @ wo


def _hier_moe(xn, wg, bg, we, be, w_gu, w_down):
    B, S, D = xn.shape
    t = xn.reshape(B * S, D)
    g_prob = jax.nn.softmax((t @ wg + bg).astype(jnp.float32), axis=-1)
    g_p, g_idx = lax.top_k(g_prob, 1)
    e_logits = (jnp.einsum('nd,gde->nge', t, we) + be).astype(jnp.float32)
    e_sel = jnp.take_along_axis(e_logits, g_idx[:, :, None], axis=1)[:, 0]
    e_prob = jax.nn.softmax(e_sel, axis=-1)
    e_p, e_idx = lax.top_k(e_prob, TOP_K_IN_GROUP)
    e_p = e_p / jnp.sum(e_p, axis=-1, keepdims=True)
    gate_w = g_p * e_p
    expert_id = g_idx * EXPERTS_PER_GROUP + e_idx
    combine = jnp.sum(jax.nn.one_hot(expert_id, N_EXPERTS, dtype=jnp.float32)
                      * gate_w[..., None], axis=1)
    out = jnp.zeros((B * S, D), jnp.float32)
    for e in range(N_EXPERTS):
        gu = t @ w_gu[e]
        h = jax.nn.silu(gu[:, :D_EXPERT]) * gu[:, D_EXPERT:]
        out = out + combine[:, e:e + 1] * (h @ w_down[e]).astype(jnp.float32)
    return out.astype(xn.dtype).reshape(B, S, D)


def setup_inputs(seed: int = 0) -> dict:
    key = jax.random.key(seed)
    ks = iter(jax.random.split(key, 32))

    def nrm(shape, scale):
        return jax.random.normal(next(ks), shape, jnp.float32) * scale

    def gain(shape):
        return 1.0 + nrm(shape, 0.02)

    u = jax.random.uniform(next(ks), (N_RG_LAYERS, D_RNN), jnp.float32, 0.9, 0.999)
    sa = u ** (1.0 / RG_C)
    rg_lambda = jnp.log(sa) - jnp.log1p(-sa)
    return {
        "x": nrm((BATCH, SEQ, D_MODEL), 1.0),
        "mem": nrm((BATCH, MEM_LEN, D_MODEL), 1.0),
        "pool_w": nrm((N_POOL_LAYERS, POOL_GROUPS, POOL_GW, POOL_GW), POOL_GW ** -0.5),
        "pool_scale": gain((N_POOL_LAYERS, D_MODEL)),
        "rg_w_in": nrm((N_RG_LAYERS, D_MODEL, 2 * D_RNN), D_MODEL ** -0.5),
        "rg_conv_w": nrm((N_RG_LAYERS, CONV_W, D_RNN), CONV_W ** -0.5),
        "rg_conv_b": nrm((N_RG_LAYERS, D_RNN), 0.01),
        "rg_w_gate": nrm((N_RG_LAYERS, RG_HEADS, RG_BLOCK, 2 * RG_BLOCK), RG_BLOCK ** -0.5),
        "rg_b_gate": nrm((N_RG_LAYERS, RG_HEADS, 2 * RG_BLOCK), 0.01),
        "rg_lambda": rg_lambda,
        "rg_w_out": nrm((N_RG_LAYERS, D_RNN, D_MODEL), D_RNN ** -0.5),
        "xa_wq": nrm((DEPTH, D_MODEL, D_MODEL), D_MODEL ** -0.5),
        "xa_wkv": nrm((DEPTH, D_MODEL, 2 * D_MODEL), D_MODEL ** -0.5),
        "xa_wo": nrm((DEPTH, D_MODEL, D_MODEL), D_MODEL ** -0.5),
        "moe_wg": nrm((DEPTH, D_MODEL, N_GROUPS), D_MODEL ** -0.5),
        "moe_bg": nrm((DEPTH, N_GROUPS), 0.01),
        "moe_we": nrm((DEPTH, N_GROUPS, D_MODEL, EXPERTS_PER_GROUP), D_MODEL ** -0.5),
        "moe_be": nrm((DEPTH, N_GROUPS, EXPERTS_PER_GROUP), 0.01),
        "moe_w_gu": nrm((DEPTH, N_EXPERTS, D_MODEL, 2 * D_EXPERT), D_MODEL ** -0.5),
        "moe_w_down": nrm((DEPTH, N_EXPERTS, D_EXPERT, D_MODEL), D_EXPERT ** -0.5),
        "norm_mix": gain((DEPTH, D_MODEL)),
        "norm_xattn": gain((DEPTH, D_MODEL)),
        "norm_mem": gain((DEPTH, D_MODEL)),
        "norm_moe": gain((DEPTH, D_MODEL)),
        "norm_final": gain((D_MODEL,)),
    }


def reference(x, mem, pool_w, pool_scale, rg_w_in, rg_conv_w, rg_conv_b, rg_w_gate, rg_b_gate,
              rg_lambda, rg_w_out, xa_wq, xa_wkv, xa_wo, moe_wg, moe_bg, moe_we, moe_be,
              moe_w_gu, moe_w_down, norm_mix, norm_xattn, norm_mem, norm_moe, norm_final):
    h = x
    for i in range(DEPTH):
        j = i // N_MIXERS
        xn = _rms_norm(h, norm_mix[i])
        if i % N_MIXERS == 0:
            h = h + _pool_mixer(xn, pool_w[j], pool_scale[j])
        else:
            h = h + _rglru_block(xn, rg_w_in[j], rg_conv_w[j], rg_conv_b[j], rg_w_gate[j],
                                 rg_b_gate[j], rg_lambda[j], rg_w_out[j])
        h = h + _mem_attention(_rms_norm(h, norm_xattn[i]), _rms_norm(mem, norm_mem[i]),
                               xa_wq[i], xa_wkv[i], xa_wo[i])
        h = h + _hier_moe(_rms_norm(h, norm_moe[i]), moe_wg[i], moe_bg[i], moe_we[i], moe_be[i],
                          moe_w_gu[i], moe_w_down[i])
    return _rms_norm(h, norm_final)
```

```python
import numpy as np
import concourse.bass as bass
import concourse.mybir as mybir
from concourse.bass_utils import run_bass_kernel_spmd

F32 = mybir.dt.float32
BF16 = mybir.dt.bfloat16
F32R = mybir.dt.float32r
AF = mybir.ActivationFunctionType
ALU = mybir.AluOpType
AX = mybir.AxisListType

D = 1024
KC = 8
TT = 512
MEM = 256
NE = 16
DE = 512
SEQ = 2048
RMS_EPS = 1e-6
NPAR = 18
SB_START = 16640
SB_END = 229376


def _esz(dt):
    return 2 if dt == BF16 else 4


class Sched:
    NSEM_DMA = 40

    def __init__(self, nc):
        self.nc = nc
        self.eng = {"pe": nc.tensor, "act": nc.scalar, "dve": nc.vector, "pool": nc.gpsimd, "sp": nc.sync}
        self.ops = []
        self.reg = {}
        self.lastw = {}
        self.readers = {}
        self.waited = {e: {} for e in self.eng}
        self.stream_pos = {e: 0 for e in self.eng}
        self.dma_count = [0] * self.NSEM_DMA
        self.dma_rr = 0
        self.cache = {}

    def register(self, handle, space, base, esz, pstep, blk):
        self.reg[handle.name] = (space, base, esz, pstep, blk)

    def blocks_of(self, ap):
        info = self.reg.get(ap.tensor.name)
        if info is None:
            return ()
        key = (ap.tensor.name, int(ap.offset), tuple(ap.ap))
        r = self.cache.get(key)
        if r is not None:
            return r
        space, base, esz, pstep, blk = info
        off = int(ap.offset) % pstep
        dims = [(abs(s), n) for (s, n) in list(ap.ap)[1:] if n > 1 and s != 0]
        dims.sort(reverse=True)
        starts = [off]
        rest = []
        for (s, n) in dims:
            if s * esz >= blk and len(starts) * n <= 512:
                starts = [st + i * s for st in starts for i in range(n)]
            else:
                rest.append((s, n))
        ext = 1 + sum(s * (n - 1) for s, n in rest)
        out = set()
        for st in starts:
            b0 = (base + st * esz) // blk
            b1 = (base + (st + ext) * esz - 1) // blk
            for b in range(b0, b1 + 1):
                out.add((space, b))
        r = tuple(out)
        self.cache[key] = r
        return r

    def add(self, eng, fn, reads, writes, dma=False):
        idx = len(self.ops)
        op = {"eng": eng, "fn": fn, "waits": [], "marked": False, "dma": dma}
        if dma:
            si = self.dma_rr
            self.dma_rr = (self.dma_rr + 1) % self.NSEM_DMA
            prev = self.dma_count[si]
            self.dma_count[si] += 1
            op["stream"] = ("dma", si)
            op["pos"] = self.dma_count[si]
            if prev > 0:
                self._want(op, ("dma", si), prev, None)
        else:
            self.stream_pos[eng] += 1
            op["stream"] = eng
            op["pos"] = self.stream_pos[eng]
        deps = {}
        rb = set()
        for ap in reads:
            rb.update(self.blocks_of(ap))
        wb = set()
        for ap in writes:
            wb.update(self.blocks_of(ap))
        for b in rb:
            w = self.lastw.get(b)
            if w is not None:
                deps[w] = True
        for b in wb:
            w = self.lastw.get(b)
            if w is not None:
                deps[w] = True
            rd = self.readers.get(b)
            if rd:
                for d in rd.values():
                    deps[d] = True
        for d in deps:
            dop = self.ops[d]
            if dop["eng"] == "pe" and eng == "pe" and not dop["dma"] and not dma:
                continue
            self._want(op, dop["stream"], dop["pos"], dop)
        for b in rb:
            if b in wb:
                continue
            rd = self.readers.get(b)
            if rd is None:
                rd = {}
                self.readers[b] = rd
            rd[op["stream"]] = idx
        for b in wb:
            self.lastw[b] = idx
            self.readers[b] = {}
        self.ops.append(op)
        return idx

    def _want(self, op, stream, pos, dop):
        w = self.waited[op["eng"]]
        if w.get(stream, 0) >= pos:
            return
        w[stream] = pos
        op["waits"].append((stream, pos))
        if dop is not None:
            dop["marked"] = True

    def finish(self, final_eng="sp"):
        op = {"eng": final_eng, "fn": None, "waits": [], "marked": False, "dma": False, "stream": final_eng,
              "pos": self.stream_pos[final_eng] + 1}
        self.stream_pos[final_eng] += 1
        for si in range(self.NSEM_DMA):
            if self.dma_count[si] > 0:
                self._want(op, ("dma", si), self.dma_count[si], None)
        last = {}
        for o in self.ops:
            if not o["dma"]:
                last[o["stream"]] = o
        for st, o in last.items():
            if st != final_eng:
                self._want(op, st, o["pos"], o)
        self.ops.append(op)

    def emit(self):
        nc = self.nc
        sems = {e: nc.alloc_semaphore("s_" + e) for e in self.eng}
        dsems = [nc.alloc_semaphore("d_%d" % i) for i in range(self.NSEM_DMA)]
        counts = {e: {} for e in self.eng}
        run = {e: 0 for e in self.eng}
        for o in self.ops:
            if o["dma"]:
                continue
            if o["marked"]:
                run[o["stream"]] += 1
            counts[o["stream"]][o["pos"]] = run[o["stream"]]
        nwait = 0
        for o in self.ops:
            e = self.eng[o["eng"]]
            wl = []
            for (st, pos) in o["waits"]:
                if isinstance(st, tuple):
                    wl.append((dsems[st[1]], 16 * pos))
                else:
                    wl.append((sems[st], counts[st][pos]))
            nwait += len(wl)
            if o["fn"] is None:
                for (s, v) in wl:
                    e.wait_ge(s, v)
                continue
            if o["dma"]:
                for (s, v) in wl:
                    e.wait_ge(s, v)
                ins = o["fn"](e)
                ins.then_inc(dsems[o["stream"][1]], 16)
            else:
                attach = bool(wl)
                for (s, v) in (wl[:-1] if attach else wl):
                    e.wait_ge(s, v)
                ins = o["fn"](e)
                if attach:
                    ins._wait_ge(wl[-1][0], wl[-1][1])
                if o["marked"]:
                    ins.then_inc(sems[o["stream"]], 1)
        return nwait


class Builder:
    def __init__(self, nseq=2, T=SEQ, phases=("load", "kv", "pool", "attn0", "moe0", "rg", "attn1", "moe1", "final")):
        self.nseq = nseq
        self.T = T
        self.NT = T // TT
        self.phases = phases
        nc = bass.Bass("TRN2", target_bir_lowering=False)
        self.nc = nc
        self.S = Sched(nc)
        self.sb_ptr = SB_START
        self.bank_rr = 0
        dt = nc.dram_tensor
        self.x = dt("x", [nseq, T, D], F32, kind="ExternalInput").ap()
        self.mem = dt("mem", [nseq, MEM, D], F32, kind="ExternalInput").ap()
        self.pool_w = dt("pool_w", [4, 256, 256], F32, kind="ExternalInput").ap()
        self.rg_w_in = dt("rg_w_in", [D, 2 * D], F32, kind="ExternalInput").ap()
        self.rg_w_gate = dt("rg_w_gate", [4, 256, 512], F32, kind="ExternalInput").ap()
        self.rg_w_out = dt("rg_w_out", [D, D], F32, kind="ExternalInput").ap()
        self.xa_wq = dt("xa_wq", [2, D, D], F32, kind="ExternalInput").ap()
        self.xa_wkv = dt("xa_wkv", [2, D, 2 * D], F32, kind="ExternalInput").ap()
        self.xa_wo = dt("xa_wo", [2, D, D], F32, kind="ExternalInput").ap()
        self.moe_w_gu = dt("moe_w_gu", [2, NE, D, 2 * DE], F32, kind="ExternalInput").ap()
        self.moe_w_down = dt("moe_w_down", [2, NE, DE, D], F32, kind="ExternalInput").ap()
        self.params_d = dt("params", [128, KC * NPAR], F32, kind="ExternalInput").ap()
        self.wr_d = dt("wr", [128, 2 * KC * 20], F32, kind="ExternalInput").ap()
        self.rbias_d = dt("rbias", [20, 2], F32, kind="ExternalInput").ap()
        self.out = dt("out", [nseq, T, D], F32, kind="ExternalOutput").ap()
        self.ps = nc.alloc_psum_tensor("ps", [128, 8, 512], F32)
        self.S.register(self.ps, "P", 0, 4, 8 * 512, 512)
        self.build()

    def alloc(self, name, shape, dtype):
        esz = _esz(dtype)
        n = 1
        for s in shape[1:]:
            n *= s
        nbytes = n * esz
        off = self.sb_ptr
        self.sb_ptr = (off + nbytes + 255) // 256 * 256
        assert self.sb_ptr <= SB_END, "SBUF overflow at %s: %d" % (name, self.sb_ptr)
        if not hasattr(self, "offs"):
            self.offs = {}
        self.offs[name] = off
        self.uid = getattr(self, "uid", 0) + 1
        t = self.nc.alloc_sbuf_tensor_at("%s_%d" % (name, self.uid), list(shape), dtype, offset=off)
        self.S.register(t, "S", off, esz, n, 256)
        return t

    def alloc_at(self, name, shape, dtype, off):
        esz = _esz(dtype)
        n = 1
        for s_ in shape[1:]:
            n *= s_
        self.uid = getattr(self, "uid", 0) + 1
        t = self.nc.alloc_sbuf_tensor_at("%s_%d" % (name, self.uid), list(shape), dtype, offset=off)
        self.S.register(t, "S", off, esz, n, 256)
        return t

    def scan(self, out, d0, d1, init):
        self.S.add("dve", lambda e: e.tensor_tensor_scan(out=out, data0=d0, data1=d1, initial=init, op0=ALU.mult, op1=ALU.add),
                   [d0, d1, init], [out])

    def mark(self):
        return self.sb_ptr

    def release(self, m):
        self.sb_ptr = m

    def bank(self):
        k = self.bank_rr
        self.bank_rr = (k + 1) % 8
        return self.ps[:, k, :]

    def mm(self, out, lhsT, rhs, start, stop):
        self.S.add("pe", lambda e: e.matmul(out, lhsT, rhs, start=start, stop=stop), [lhsT, rhs], [out])

    def tr(self, out, in_, ident):
        self.S.add("pe", lambda e: e.transpose(out, in_, ident), [in_, ident], [out])

    def const_ap(self, val, npart=128):
        cc = getattr(self, "_consts", None)
        if cc is None:
            cc = self._consts = {}
        if val not in cc:
            t = self.alloc("c%d" % len(cc), [128, 1], F32)
            self.memset("pool", t[:], float(val))
            cc[val] = t
        return cc[val][0:npart, :]

    def act(self, out, in_, func, bias=None, scale=None):
        reads = [in_]
        kw = {}
        if isinstance(bias, (int, float)):
            bias = self.const_ap(float(bias), in_.shape[0])
        if bias is not None:
            kw["bias"] = bias
            if not isinstance(bias, (int, float)):
                reads.append(bias)
        if scale is not None:
            kw["scale"] = scale
            if not isinstance(scale, (int, float)):
                reads.append(scale)
        self.S.add("act", lambda e: e.activation(out=out, in_=in_, func=func, **kw), reads, [out])

    def copy(self, eng, out, in_):
        if eng == "act":
            self.act(out, in_, AF.Copy)
        else:
            self.S.add(eng, lambda e: e.tensor_copy(out=out, in_=in_), [in_], [out])

    def tt(self, eng, out, in0, in1, op):
        self.S.add(eng, lambda e: e.tensor_tensor(out=out, in0=in0, in1=in1, op=op), [in0, in1], [out])

    def ts(self, eng, out, in0, s1, s2, op0, op1=None):
        reads = [in0] + [s for s in (s1, s2) if s is not None and not isinstance(s, (int, float))]
        if op1 is None:
            self.S.add(eng, lambda e: e.tensor_scalar(out=out, in0=in0, scalar1=s1, scalar2=None, op0=op0), reads, [out])
        else:
            self.S.add(eng, lambda e: e.tensor_scalar(out=out, in0=in0, scalar1=s1, scalar2=s2, op0=op0, op1=op1), reads, [out])

    def stt(self, eng, out, in0, scalar, in1, op0, op1):
        reads = [in0, in1] + ([] if isinstance(scalar, (int, float)) else [scalar])
        self.S.add(eng, lambda e: e.scalar_tensor_tensor(out=out, in0=in0, scalar=scalar, in1=in1, op0=op0, op1=op1), reads, [out])

    def red(self, eng, out, in_, op):
        self.S.add(eng, lambda e: e.tensor_reduce(out=out, in_=in_, axis=AX.X, op=op), [in_], [out])

    def recip(self, out, in_):
        self.S.add("dve", lambda e: e.reciprocal(out=out, in_=in_), [in_], [out])

    def memset(self, eng, ap, val):
        self.S.add(eng, lambda e: e.memset(ap, val), [], [ap])

    def dma(self, q, out, in_):
        self.S.add(q, lambda e: e.dma_start(out=out, in_=in_), [in_], [out], dma=True)

    def setup_consts(self):
        nc = self.nc
        self.ident_f = self.alloc("ident_f", [128, 128], F32)
        self.ident_b = self.alloc("ident_b", [128, 128], BF16)
        self.ones_f = self.alloc("ones_f", [128, 128], F32)
        self.ones_b = self.alloc("ones_b", [128, 128], BF16)
        self.sel = self.alloc("sel", [16, NE, 128], F32R)
        self.params = self.alloc("params", [128, KC, NPAR], F32)
        self.wr = self.alloc("wr", [128, 2, KC, 20], F32)
        self.wrg = self.alloc("wrg", [128, 2, KC, 20], F32)
        self.rbias = self.alloc("rbias", [20, 2], F32)
        self.nsp = self.alloc("nsp", [128, KC], F32)
        self.nsp2 = self.alloc("nsp2", [128, KC], F32)
        self.ic = self.alloc("ic", [128, 16], F32)
        self.memset("pool", self.ident_f[:], 0.0)
        idf = self.ident_f
        self.S.add("pool", lambda e: e.affine_select(out=idf[:], in_=idf[:], pattern=[[-1, 128]], compare_op=ALU.not_equal,
                                                     fill=1.0, base=0, channel_multiplier=1), [idf[:]], [idf[:]])
        self.copy("dve", self.ident_b[:], self.ident_f[:])
        self.memset("dve", self.ones_f[:], 1.0)
        self.ones_r = self.alloc("ones_r", [128, 128], F32R)
        self.copy("act", self.ones_r[:], self.ones_f[:])
        self.memset("dve", self.ones_b[:], 1.0)
        self.copy("act", self.sel[:], self.ident_f[0:16, 0:16].unsqueeze(2).to_broadcast([16, NE, 128]))
        self.dma("sp", self.params[:], self.params_d.rearrange("p (c r) -> p c r", c=KC))
        self.dma("sp", self.wr[:], self.wr_d.rearrange("p (l c j) -> p l c j", l=2, c=KC))
        self.dma("sp", self.rbias[:], self.rbias_d)
        for l in range(2):
            for c in range(KC):
                self.ts("dve", self.wrg[:, l, c, :], self.wr[:, l, c, :], self.par(6 + l, c), None, ALU.mult)
        lam = self.params[:, :, 15]
        self.act(self.nsp[:], lam, AF.Exp, scale=-1.0)
        self.act(self.nsp[:], self.nsp[:], AF.Ln, bias=1.0)
        self.ts("dve", self.nsp2[:], self.nsp[:], -16.0, None, ALU.mult)
        self.nsph = self.alloc("nsph", [128, KC], F32)
        self.ts("dve", self.nsph[:], self.nsp[:], -4.0, None, ALU.mult)
        self.ts("dve", self.nsp[:], self.nsp[:], -8.0, None, ALU.mult)
        self.hb = self.alloc("hb", [128, KC, 2], F32)
        self.ts("dve", self.hb[:], self.params[:, :, 16:18], 0.5, None, ALU.mult)
        for t in range(16):
            self.memset("pool", self.ic[:, t:t + 1], 1.0 / (t + 1))

    def par(self, r, c):
        return self.params[:, c, r:r + 1]

    def alloc_norm_scratch(self, nsq=4, nrstd=2):
        self.sq = [self.alloc("sq%d" % i, [128, TT], F32R) for i in range(nsq)]
        self.std = self.alloc("std", [128, TT], F32)
        self.rstd = [self.alloc("rstd%d" % i, [128, TT], F32) for i in range(nrstd)]
        self.norm_rr = 0

    def norm_stats(self, chunks, N):
        bank = self.bank()
        for c in range(KC):
            sq = self.sq[c % len(self.sq)]
            self.act(sq[:, 0:N], chunks[c], AF.Square)
            self.mm(bank[:, 0:N], self.ones_r[:], sq[:, 0:N], start=(c == 0), stop=(c == KC - 1))
        self.act(self.std[:, 0:N], bank[:, 0:N], AF.Ln, bias=self.eps_ap, scale=1.0 / D)
        r = self.rstd[self.norm_rr % len(self.rstd)]
        self.norm_rr += 1
        self.act(r[:, 0:N], self.std[:, 0:N], AF.Exp, scale=-0.5)
        return r[:, 0:N]

    def norm_apply(self, chunks, rstd, grow, outs, eng="dve"):
        for c in range(KC):
            self.stt(eng, outs[c], chunks[c], self.par(grow, c), rstd, ALU.mult, ALU.mult)

    def h_chunks(self, tt):
        return [self.h[:, c, tt * TT:(tt + 1) * TT] for c in range(KC)]

    def phase_load(self, b):
        m = self.mark()
        xs = [self.alloc("xs%d" % i, [128, D], F32) for i in range(2)]
        for i in range(self.T // 128):
            xsb = xs[i % 2]
            self.dma("sp", xsb[:], self.x[b, i * 128:(i + 1) * 128, :])
            for half in range(2):
                bank = self.bank()
                for cc in range(4):
                    c = half * 4 + cc
                    self.tr(bank[:, cc * 128:(cc + 1) * 128], xsb[:, c * 128:(c + 1) * 128], self.ident_f[:])
                self.copy("act" if half == 0 else "dve", self.h[:, half * 4:half * 4 + 4, i * 128:(i + 1) * 128],
                          bank.rearrange("p (c t) -> p c t", c=4))
        self.release(m)

    def phase_final(self, b, do_norm=True):
        m = self.mark()
        self.alloc_norm_scratch()
        hn = self.alloc("hn", [128, KC, TT], F32)
        ot = [self.alloc("ot%d" % i, [128, D], F32) for i in range(2)]
        k = 0
        for tt in range(self.NT):
            hc = self.h_chunks(tt)
            if do_norm:
                rstd = self.norm_stats(hc, TT)
                self.norm_apply(hc, rstd, 8, [hn[:, c, :] for c in range(KC)])
                src = [hn[:, c, :] for c in range(KC)]
            else:
                src = hc
            for s in range(4):
                o = ot[k % 2]
                k += 1
                for half in range(2):
                    bank = self.bank()
                    for cc in range(4):
                        c = half * 4 + cc
                        self.tr(bank[:, cc * 128:(cc + 1) * 128], src[c][:, s * 128:(s + 1) * 128], self.ident_f[:])
                    self.copy("act" if half == 0 else "dve", o[:, half * 512:(half + 1) * 512], bank)
                t0 = tt * TT + s * 128
                self.dma("sp", self.out[b, t0:t0 + 128, :], o[:])
        self.release(m)

    def load_w(self, dst, src, kchunks, ncols, split_cols=1024):
        sv = src.rearrange("(k p) n -> p k n", p=128)
        for c0 in range(0, ncols, split_cols):
            c1 = min(ncols, c0 + split_cols)
            self.dma("pool", dst[:, :, c0:c1], sv[:, :, c0:c1])

    def phase_kv(self, b):
        m = self.mark()
        self.alloc_norm_scratch()
        wkv = self.alloc("wkv", [128, KC, 2 * D], BF16)
        ms = [self.alloc("ms%d" % i, [128, D], F32) for i in range(2)]
        memT = self.alloc("memT", [128, KC, MEM], F32)
        memn = self.alloc("memn", [128, KC, MEM], BF16)
        for i in range(2):
            self.dma("sp", ms[i][:], self.mem[b, i * 128:(i + 1) * 128, :])
            for half in range(2):
                bank = self.bank()
                for cc in range(4):
                    c = half * 4 + cc
                    self.tr(bank[:, cc * 128:(cc + 1) * 128], ms[i][:, c * 128:(c + 1) * 128], self.ident_f[:])
                self.copy("act" if half == 0 else "dve", memT[:, half * 4:half * 4 + 4, i * 128:(i + 1) * 128],
                          bank.rearrange("p (c t) -> p c t", c=4))
        for l in range(2):
            self.load_w(wkv, self.xa_wkv[l], KC, 2 * D)
            mc_ = [memT[:, c, :] for c in range(KC)]
            rstd = self.norm_stats(mc_, MEM)
            self.norm_apply(mc_, rstd, 4 + l, [memn[:, c, :] for c in range(KC)])
            for o in range(KC):
                bank = self.bank()
                for c in range(KC):
                    self.mm(bank[:, 0:MEM], wkv[:, c, o * 128:(o + 1) * 128], memn[:, c, :], start=(c == 0), stop=(c == KC - 1))
                self.copy("act" if o % 2 == 0 else "dve", self.kT[l][:, o, :], bank[:, 0:MEM])
            for mc in range(2):
                for n in range(2):
                    bank = self.bank()
                    for c in range(KC):
                        self.mm(bank, memn[:, c, mc * 128:(mc + 1) * 128], wkv[:, c, D + n * 512:D + (n + 1) * 512],
                                start=(c == 0), stop=(c == KC - 1))
                    self.copy("act" if n % 2 == 0 else "dve", self.v[l][:, mc, n * 512:(n + 1) * 512], bank)
        self.release(m)

    def phase_boundary(self, b_fin, b_next, do_norm=True, do_kv=True):
        m = self.mark()
        self.alloc_norm_scratch()
        pieces_f, pieces_k, pieces_l = [], [], []
        if b_fin is not None:
            hn = self.alloc("hn", [128, KC, TT], F32)
            ot = [self.alloc("ot%d" % i, [128, D], F32) for i in range(2)]
            cnt = [0]

            def fin_tile(tt, b=b_fin):
                hc = self.h_chunks(tt)
                if do_norm:
                    rstd = self.norm_stats(hc, TT)
                    self.norm_apply(hc, rstd, 8, [hn[:, c, :] for c in range(KC)])
                    src = [hn[:, c, :] for c in range(KC)]
                else:
                    src = hc
                for s_ in range(4):
                    o = ot[cnt[0] % 2]
                    cnt[0] += 1
                    for half in range(2):
                        bank = self.bank()
                        for cc in range(4):
                            c = half * 4 + cc
                            self.tr(bank[:, cc * 128:(cc + 1) * 128], src[c][:, s_ * 128:(s_ + 1) * 128], self.ident_f[:])
                        self.copy("act" if half == 0 else "dve", o[:, half * 512:(half + 1) * 512], bank)
                    t0 = tt * TT + s_ * 128
                    self.dma("sp", self.out[b, t0:t0 + 128, :], o[:])

            pieces_f = [(lambda tt=tt: fin_tile(tt)) for tt in range(self.NT)]
        if b_next is not None:
            xs = [self.alloc("xs%d" % i, [128, D], F32) for i in range(2)]

            def load_tiles(i0, i1, b=b_next):
                for i in range(i0, i1):
                    xsb = xs[i % 2]
                    self.dma("sp", xsb[:], self.x[b, i * 128:(i + 1) * 128, :])
                    for half in range(2):
                        bank = self.bank()
                        for cc in range(4):
                            c = half * 4 + cc
                            self.tr(bank[:, cc * 128:(cc + 1) * 128], xsb[:, c * 128:(c + 1) * 128], self.ident_f[:])
                        self.copy("act" if half == 0 else "dve", self.h[:, half * 4:half * 4 + 4, i * 128:(i + 1) * 128],
                                  bank.rearrange("p (c t) -> p c t", c=4))

            n128 = self.T // 128
            q4 = max(1, n128 // 4)
            pieces_l = [(lambda i0=i0: load_tiles(i0, min(n128, i0 + q4))) for i0 in range(0, n128, q4)]
            if do_kv:
                wkv = self.alloc("wkv", [128, KC, 2 * D], BF16)
                ms = [self.alloc("ms%d" % i, [128, D], F32) for i in range(2)]
                memT = self.alloc("memT", [128, KC, MEM], F32)
                memn = self.alloc("memn", [128, KC, MEM], BF16)
                bn = b_next

                def k0():
                    for i in range(2):
                        self.dma("sp", ms[i][:], self.mem[bn, i * 128:(i + 1) * 128, :])
                    self.load_w(wkv, self.xa_wkv[0], KC, 2 * D)

                def k1():
                    for i in range(2):
                        for half in range(2):
                            bank = self.bank()
                            for cc in range(4):
                                c = half * 4 + cc
                                self.tr(bank[:, cc * 128:(cc + 1) * 128], ms[i][:, c * 128:(c + 1) * 128], self.ident_f[:])
                            self.copy("act" if half == 0 else "dve", memT[:, half * 4:half * 4 + 4, i * 128:(i + 1) * 128],
                                      bank.rearrange("p (c t) -> p c t", c=4))

                def kk(l):
                    mc_ = [memT[:, c, :] for c in range(KC)]
                    rstd = self.norm_stats(mc_, MEM)
                    self.norm_apply(mc_, rstd, 4 + l, [memn[:, c, :] for c in range(KC)])
                    for o in range(KC):
                        bank = self.bank()
                        for c in range(KC):
                            self.mm(bank[:, 0:MEM], wkv[:, c, o * 128:(o + 1) * 128], memn[:, c, :], start=(c == 0), stop=(c == KC - 1))
                        self.copy("act" if o % 2 == 0 else "dve", self.kT[l][:, o, :], bank[:, 0:MEM])

                def kv_(l):
                    for mc in range(2):
                        for n in range(2):
                            bank = self.bank()
                            for c in range(KC):
                                self.mm(bank, memn[:, c, mc * 128:(mc + 1) * 128], wkv[:, c, D + n * 512:D + (n + 1) * 512],
                                        start=(c == 0), stop=(c == KC - 1))
                            self.copy("act" if n % 2 == 0 else "dve", self.v[l][:, mc, n * 512:(n + 1) * 512], bank)
                    if l == 0:
                        self.load_w(wkv, self.xa_wkv[1], KC, 2 * D)

                pieces_k = [k0, k1, lambda: kk(0), lambda: kv_(0), lambda: kk(1), lambda: kv_(1)]
        order = []
        pk = list(pieces_k)
        pf = list(pieces_f)
        pl = list(pieces_l)
        if pk:
            order.append(pk.pop(0))
        while pf or pk or pl:
            if pf:
                order.append(pf.pop(0))
            if pk:
                order.append(pk.pop(0))
            if not pf and pl:
                order.append(pl.pop(0))
        for fn in order:
            fn()
        self.release(m)

    def phase_attn(self, b, l, prefetch_moe=False):
        m = self.mark()
        if prefetch_moe:
            pwgu, pwdn = self.moe_slots(1)
        self.alloc_norm_scratch(nsq=2)
        wq = self.alloc("wq", [128, KC, D], BF16)
        wo = self.alloc("wo", [128, KC, D], BF16)
        self.load_w(wq, self.xa_wq[l], KC, D)
        self.load_w(wo, self.xa_wo[l], KC, D)
        if prefetch_moe:
            self.load_expert(l, 0, pwgu, pwdn)
        xn = [self.alloc("xn_t%d" % i, [128, KC, TT], BF16) for i in range(2)]
        qT = [self.alloc("qT%d" % i, [128, KC, TT], BF16) for i in range(2)]
        oT = [self.alloc("oT%d" % i, [128, KC, TT], BF16) for i in range(1)]
        E = [self.alloc("E%d" % i, [128, 2, TT], BF16) for i in range(2)]
        rden = [self.alloc("rden%d" % i, [128, TT], F32) for i in range(1)]
        kT, v = self.kT[l], self.v[l]

        def N(tt):
            hc = self.h_chunks(tt)
            rstd = self.norm_stats(hc, TT)
            self.norm_apply(hc, rstd, 2 + l, [xn[tt % 2][:, c, :] for c in range(KC)])

        def Q(tt):
            x_, q_ = xn[tt % 2], qT[tt % 2]
            for o in range(KC):
                bank = self.bank()
                for c in range(KC):
                    self.mm(bank, wq[:, c, o * 128:(o + 1) * 128], x_[:, c, :], start=(c == 0), stop=(c == KC - 1))
                self.act(q_[:, o, :], bank, AF.Copy, scale=1.0 / 16.0)

        def H(tt):
            q_, o_ = qT[tt % 2], oT[0]

            def scores(hd):
                e_ = E[hd % 2]
                for mc in range(2):
                    bank = self.bank()
                    for dc in range(2):
                        self.mm(bank, kT[:, 2 * hd + dc, mc * 128:(mc + 1) * 128], q_[:, 2 * hd + dc, :],
                                start=(dc == 0), stop=(dc == 1))
                    self.act(e_[:, mc, :], bank, AF.Exp)

            def pv(hd):
                e_ = E[hd % 2]
                r_ = rden[0]
                bank = self.bank()
                for mc in range(2):
                    self.mm(bank, self.ones_b[:], e_[:, mc, :], start=(mc == 0), stop=(mc == 1))
                self.act(r_[:], bank, AF.Ln)
                self.act(r_[:], r_[:], AF.Exp, scale=-1.0)
                for dc in range(2):
                    bank = self.bank()
                    for mc in range(2):
                        self.mm(bank, v[:, mc, (2 * hd + dc) * 128:(2 * hd + dc + 1) * 128], e_[:, mc, :],
                                start=(mc == 0), stop=(mc == 1))
                    self.tt("dve", o_[:, 2 * hd + dc, :], bank, r_[:], ALU.mult)

            scores(0)
            for hd in range(4):
                if hd + 1 < 4:
                    scores(hd + 1)
                pv(hd)

        def O(tt):
            hc = self.h_chunks(tt)
            o_ = oT[0]
            for o in range(KC):
                bank = self.bank()
                for c in range(KC):
                    self.mm(bank, wo[:, c, o * 128:(o + 1) * 128], o_[:, c, :], start=(c == 0), stop=(c == KC - 1))
                self.tt("dve", hc[o], hc[o], bank, ALU.add)

        N(0)
        Q(0)
        for tt in range(self.NT):
            if tt + 1 < self.NT:
                N(tt + 1)
            H(tt)
            if tt + 1 < self.NT:
                Q(tt + 1)
            O(tt)
        self.release(m)

    def phase_pool(self, b):
        m = self.mark()
        self.alloc_norm_scratch()
        W = 16 + TT
        pw = self.alloc("pw", [128, 4, 2, 256], BF16)
        self.dma("pool", pw[:], self.pool_w.rearrange("g (kc p) o -> p g kc o", p=128))
        xnfs = [self.alloc("xnf%d" % i, [128, KC, W], F32) for i in range(2)]
        lv = [[self.alloc("lv%d%d" % (g, k), [128, 2, W], F32) for k in range(g + 1)] for g in range(4)]
        pb = self.alloc("pb", [128, KC, TT], BF16)
        fix = [self.alloc("fix%d" % g, [128, 2, 16], F32) for g in range(4)]
        for tt in range(self.NT):
            hc = self.h_chunks(tt)
            xnf = xnfs[tt % 2]
            if tt == 0:
                self.memset("dve", xnf[:, :, 0:16], 0.0)
            else:
                self.copy("act", xnf[:, :, 0:16], xnfs[(tt - 1) % 2][:, :, TT:TT + 16])
            rstd = self.norm_stats(hc, TT)
            xgs = [xnf[:, 2 * g:2 * g + 2, :] for g in range(4)]

            def prep(g):
                xg = xgs[g]
                for cc in range(2):
                    c = 2 * g + cc
                    self.stt("dve", xnf[:, c, 16:W], hc[c], self.par(0, c), rstd, ALU.mult, ALU.mult)

            def levels(g, eng):
                src = xgs[g]
                lo = 0
                for k in range(g + 1):
                    sh = 1 << k
                    nlo = lo + sh
                    dst = lv[g][k]
                    self.tt(eng, dst[:, :, nlo:W], src[:, :, nlo:W], src[:, :, lo:W - sh], ALU.add)
                    src = dst
                    lo = nlo
                return src

            def finish(g, src):
                xg = xgs[g]
                w = 2 << g
                self.stt("dve", pb[:, 2 * g:2 * g + 2, :], src[:, :, 16:W], 1.0 / w, xg[:, :, 16:W], ALU.mult, ALU.subtract)
                if tt == 0:
                    nf = w - 1
                    self.tt("dve", fix[g][:, :, 0:nf], src[:, :, 16:16 + nf],
                            self.ic[:, 0:nf].unsqueeze(1).to_broadcast([128, 2, nf]), ALU.mult)
                    self.tt("dve", pb[:, 2 * g:2 * g + 2, 0:nf], fix[g][:, :, 0:nf], xg[:, :, 16:16 + nf], ALU.subtract)
                banks = []
                for oc in range(2):
                    bank = self.bank()
                    for kc in range(2):
                        self.mm(bank, pw[:, g, kc, oc * 128:(oc + 1) * 128], pb[:, 2 * g + kc, :], start=(kc == 0), stop=(kc == 1))
                    banks.append(bank)
                return banks

            def update(g, banks):
                for oc in range(2):
                    c = 2 * g + oc
                    self.stt("dve", hc[c], banks[oc], self.par(9, c), hc[c], ALU.mult, ALU.add)

            pend = None
            for g in range(4):
                prep(g)
                bk = finish(g, levels(g, "dve"))
                if pend is not None:
                    update(*pend)
                pend = (g, bk)
            update(*pend)
        self.release(m)

    def phase_rg(self, b):
        m = self.mark()
        self.alloc_norm_scratch(nsq=2)
        w_in = self.alloc("w_in", [128, KC, 2 * D], BF16)
        w_gate = self.alloc("w_gate", [128, 4, 2, 512], BF16)
        w_out = self.alloc("w_out", [128, KC, D], BF16)
        self.load_w(w_in, self.rg_w_in, KC, 2 * D)
        self.dma("pool", w_gate[:], self.rg_w_gate.rearrange("h (kc p) o -> p h kc o", p=128))
        self.load_w(w_out, self.rg_w_out, KC, D)
        xbh = self.alloc("xbh", [128, KC, 4], F32)
        st = self.alloc("st", [128, KC], F32)
        self.memset("pool", xbh[:], 0.0)
        self.memset("pool", st[:], 0.0)
        xns = [self.alloc("xn_t", [128, KC, TT], BF16), self.alloc_at("xn_t2", [128, KC, TT], BF16, self.offs["sel"])]
        y_t = self.alloc("y_t", [128, KC, TT], BF16)
        xbp = [[self.alloc("xbp%d%d" % (i, j), [128, 3 + TT], F32) for j in range(2)] for i in range(2)]
        xc = [[self.alloc("xc%d%d" % (i, j), [128, TT], F32) for j in range(2)] for i in range(2)]
        xcb_all = self.alloc_at("xcb", [128, 2, 2, TT], BF16, self.offs["kT0"])
        R = [self.alloc("R%d" % i, [128, TT], F32) for i in range(2)]
        I = [self.alloc("I%d" % i, [128, TT], F32) for i in range(2)]
        A = [self.alloc("A%d" % i, [128, TT], F32) for i in range(2)]

        cur = {"xn": xns[0]}

        def stageA(hd):
            xn = cur["xn"]
            hp = hd % 2
            for cc in range(2):
                c = 2 * hd + cc
                bank = self.bank()
                for k in range(KC):
                    self.mm(bank, w_in[:, k, c * 128:(c + 1) * 128], xn[:, k, :], start=(k == 0), stop=(k == KC - 1))
                xb = xbp[hp][cc]
                self.copy("pool", xb[:, 0:3], xbh[:, c, 0:3])
                self.copy("act", xb[:, 3:3 + TT], bank)
                x_ = xc[hp][cc]
                self.ts("dve", x_[:], xb[:, 3:3 + TT], self.par(13, c), self.par(14, c), ALU.mult, ALU.add)
                self.copy("pool", xbh[:, c, 0:3], xb[:, TT:TT + 3])
                for k in range(3):
                    self.stt("dve", x_[:], xb[:, k:k + TT], self.par(10 + k, c), x_[:], ALU.mult, ALU.add)
                self.copy("dve", xcb_all[:, hp, cc, :], x_[:])

        def stageB(hd):
            xn = cur["xn"]
            hp = hd % 2
            gb = []
            for oc in range(4):
                bank = self.bank()
                for kc in range(2):
                    self.mm(bank, w_gate[:, hd, kc, oc * 128:(oc + 1) * 128], xcb_all[:, hp, kc, :], start=(kc == 0), stop=(kc == 1))
                gb.append(bank)
            yb = []
            for cc in range(2):
                c = 2 * hd + cc
                bank = self.bank()
                for k in range(KC):
                    self.mm(bank, w_in[:, k, D + c * 128:D + (c + 1) * 128], xn[:, k, :], start=(k == 0), stop=(k == KC - 1))
                yb.append(bank)
            for cc in range(2):
                c = 2 * hd + cc
                self.act(R[cc][:], gb[cc], AF.Sigmoid, bias=self.par(16, c))
                self.act(I[cc][:], gb[2 + cc], AF.Sigmoid, bias=self.par(17, c))
            for cc in range(2):
                self.tt("dve", I[cc][:], I[cc][:], xc[hp][cc][:], ALU.mult)
            for cc in range(2):
                c = 2 * hd + cc
                self.act(A[cc][:], R[cc][:], AF.Exp, scale=self.nsp[:, c:c + 1])
                self.act(R[cc][:], R[cc][:], AF.Exp, scale=self.nsp2[:, c:c + 1])
            for cc in range(2):
                self.act(R[cc][:], R[cc][:], AF.Sqrt, bias=1.0, scale=-1.0)
            for cc in range(2):
                c = 2 * hd + cc
                self.tt("dve", I[cc][:], I[cc][:], R[cc][:], ALU.mult)
                stc = st[:, c:c + 1]
                self.scan(R[cc][:], A[cc][:], I[cc][:], stc)
                self.copy("dve", stc, R[cc][:, TT - 1:TT])
            for cc in range(2):
                self.act(xc[hp][cc][:], yb[cc], AF.Gelu_apprx_tanh)
            for cc in range(2):
                c = 2 * hd + cc
                self.tt("dve", y_t[:, c, :], R[cc][:], xc[hp][cc][:], ALU.mult)

        def NRM(tt):
            hc_ = self.h_chunks(tt)
            rstd = self.norm_stats(hc_, TT)
            self.norm_apply(hc_, rstd, 1, [xns[tt % 2][:, c, :] for c in range(KC)])

        NRM(0)
        cur["xn"] = xns[0]
        stageA(0)
        for tt in range(self.NT):
            hc = self.h_chunks(tt)
            for hd in range(4):
                cur["xn"] = xns[tt % 2]
                if hd + 1 < 4:
                    stageA(hd + 1)
                if hd == 3 and tt + 1 < self.NT:
                    NRM(tt + 1)
                stageB(hd)
            if tt + 1 < self.NT:
                cur["xn"] = xns[(tt + 1) % 2]
                stageA(0)
            for o in range(KC):
                bank = self.bank()
                for c in range(KC):
                    self.mm(bank, w_out[:, c, o * 128:(o + 1) * 128], y_t[:, c, :], start=(c == 0), stop=(c == KC - 1))
                self.tt("dve", hc[o], hc[o], bank, ALU.add)
        self.release(m)

    def moe_slots(self, n=2):
        wgu, wdn = [], []
        for i in range(n):
            wgu.append(self.alloc("wgu%d" % i, [128, KC, 2 * DE], BF16))
            wdn.append(self.alloc("wdn%d" % i, [128, 4, D], BF16))
        return wgu, wdn

    def load_expert(self, l, e, wgu, wdn):
        s = e % 2
        self.load_w(wgu[s], self.moe_w_gu[l, e], KC, 2 * DE)
        self.load_w(wdn[s], self.moe_w_down[l, e], 4, D)

    def phase_moe(self, b, l, prefetched0=False):
        m = self.mark()
        T, NT = self.T, self.NT
        wgu, wdn = self.moe_slots()
        xn = self.alloc("xn_full", [128, KC, T], BF16)
        cT = self.alloc("cT", [16, T], F32R)
        if not prefetched0:
            self.load_expert(l, 0, wgu, wdn)
        self.load_expert(l, 1, wgu, wdn)
        self.copy("act", self.sel[:], self.ident_f[0:16, 0:16].unsqueeze(2).to_broadcast([16, NE, 128]))
        self.alloc_norm_scratch(nsq=2, nrstd=1)
        lt = self.alloc("lt", [20, TT], F32)
        Ls = self.alloc("Ls", [128, 4, 20], F32)
        sm = {}
        for nm, shp in (("gmax", [128, 4]), ("gsh", [128, 4, 4]), ("gsum", [128, 4]), ("gp", [128, 4]), ("gm", [128, 4, 4]),
                        ("m1", [128, 4, 4]), ("esh", [128, 4, 4, 4]), ("mask1", [128, 4, 4, 4]), ("m2", [128, 4, 4]),
                        ("top2", [128, 4, 4, 4]), ("eex", [128, 4, 4, 4]), ("den", [128, 4, 4]), ("comb", [128, 4, 16])):
            sm[nm] = self.alloc(nm, shp, F32)
        sg = [self.alloc("sg%d" % i, [128, TT], F32) for i in range(2)]
        t1 = [self.alloc("t1%d" % i, [128, TT], F32) for i in range(1)]
        cb = [self.alloc("cb%d" % i, [128, TT], F32) for i in range(2)]
        hh = [self.alloc_at("hh0", [128, 4, TT], BF16, self.offs["kT0"]),
              self.alloc_at("hh1", [128, 4, TT], BF16, self.offs["v0"])]
        state = {}

        def NRa(tt):
            hc = self.h_chunks(tt)
            rstd = self.norm_stats(hc, TT)
            self.norm_apply(hc, rstd, 6 + l, [xn[:, c, tt * TT:(tt + 1) * TT] for c in range(KC)])
            bank = self.bank()
            for c in range(KC):
                self.mm(bank[0:20, :], self.wrg[:, l, c, :], hc[c], start=(c == 0), stop=(c == KC - 1))
            self.tt("dve", lt[:], bank[0:20, :], rstd[0:20, :], ALU.mult)
            self.act(lt[:], lt[:], AF.Identity, bias=self.rbias[:, l:l + 1])

        def NRb(tt):
            bankL = self.bank()
            for s in range(4):
                self.tr(bankL[:, s * 20:(s + 1) * 20], lt[0:20, s * 128:(s + 1) * 128], self.ident_f[0:20, 0:20])
            self.copy("act", Ls[:], bankL[:, 0:80].rearrange("p (s j) -> p s j", s=4))
            gl = Ls[:, :, 0:4]
            el = Ls[:, :, 4:20].rearrange("p s (g e) -> p s g e", g=4)
            bc3 = lambda ap: ap.unsqueeze(2).to_broadcast([128, 4, 4])
            bc4 = lambda ap: ap.unsqueeze(3).to_broadcast([128, 4, 4, 4])
            self.red("dve", sm["gmax"][:], gl, ALU.max)
            self.tt("dve", sm["gsh"][:], gl, bc3(sm["gmax"][:]), ALU.subtract)
            self.ts("dve", sm["gm"][:], sm["gsh"][:], 0.0, None, ALU.is_ge)
            self.act(sm["gsh"][:], sm["gsh"][:], AF.Exp)
            self.red("dve", sm["gsum"][:], sm["gsh"][:], ALU.add)
            self.recip(sm["gp"][:], sm["gsum"][:])
            self.tt("dve", sm["gm"][:], sm["gm"][:], bc3(sm["gp"][:]), ALU.mult)
            self.red("dve", sm["m1"][:], el, ALU.max)
            self.tt("dve", sm["esh"][:], el, bc4(sm["m1"][:]), ALU.subtract)
            self.ts("dve", sm["mask1"][:], sm["esh"][:], 0.0, None, ALU.is_ge)
            self.stt("dve", sm["mask1"][:], sm["mask1"][:], -1e30, sm["esh"][:], ALU.mult, ALU.add)
            self.red("dve", sm["m2"][:], sm["mask1"][:], ALU.max)
            self.tt("dve", sm["top2"][:], sm["esh"][:], bc4(sm["m2"][:]), ALU.is_ge)
            self.act(sm["eex"][:], sm["esh"][:], AF.Exp)
            self.tt("dve", sm["eex"][:], sm["eex"][:], sm["top2"][:], ALU.mult)
            self.red("dve", sm["den"][:], sm["eex"][:], ALU.add)
            self.recip(sm["den"][:], sm["den"][:])
            self.tt("dve", sm["den"][:], sm["den"][:], sm["gm"][:], ALU.mult)
            self.tt("dve", sm["comb"][:].rearrange("p s (g e) -> p s g e", g=4), sm["eex"][:], bc4(sm["den"][:]), ALU.mult)

        def NRc(tt):
            bankC = self.bank()
            for s in range(4):
                self.tr(bankC[0:16, s * 128:(s + 1) * 128], sm["comb"][:, s, :], self.ident_f[:])
            self.copy("act", cT[:, tt * TT:(tt + 1) * TT], bankC[0:16, :])

        seq = [(e, tt) for e in range(NE) for tt in range(NT)]

        def gu(q):
            e, tt = seq[q]
            s = e % 2
            par = q % 2
            bank = self.bank()
            self.mm(bank, self.sel[:, e, :], cT[:, tt * TT:(tt + 1) * TT], start=True, stop=True)
            self.copy("act", cb[par][:], bank)
            for j in range(4):
                bg = self.bank()
                for k in range(KC):
                    self.mm(bg, wgu[s][:, k, j * 128:(j + 1) * 128], xn[:, k, tt * TT:(tt + 1) * TT], start=(k == 0), stop=(k == KC - 1))
                bu = self.bank()
                for k in range(KC):
                    self.mm(bu, wgu[s][:, k, DE + j * 128:DE + (j + 1) * 128], xn[:, k, tt * TT:(tt + 1) * TT], start=(k == 0), stop=(k == KC - 1))
                self.act(sg[j % 2][:], bg, AF.Silu)
                self.tt("dve", t1[0][:], sg[j % 2][:], bu, ALU.mult)
                self.tt("dve", hh[par][:, j, :], t1[0][:], cb[par][:], ALU.mult)

        def down(q):
            e, tt = seq[q]
            s = e % 2
            par = q % 2
            hc = self.h_chunks(tt)
            for mo in range(KC):
                bank = self.bank()
                for j in range(4):
                    self.mm(bank, wdn[s][:, j, mo * 128:(mo + 1) * 128], hh[par][:, j, :], start=(j == 0), stop=(j == 3))
                self.tt("dve", hc[mo], hc[mo], bank, ALU.add)
            if tt == NT - 1 and e + 2 < NE:
                self.load_expert(l, e + 2, wgu, wdn)

        NRa(0); NRb(0); NRc(0)
        for q in range(len(seq)):
            e, tt = seq[q]
            nxt = tt + 1
            if e == 0 and nxt < NT:
                NRa(nxt)
            gu(q)
            if e == 0 and nxt < NT:
                NRb(nxt)
            if q > 0:
                down(q - 1)
            if e == 0 and nxt < NT:
                NRc(nxt)
        down(len(seq) - 1)
        self.release(m)

    def build(self):
        T = self.T
        self.eps_t = self.alloc("eps", [128, 1], F32)
        self.eps_ap = self.eps_t[:]
        self.memset("dve", self.eps_t[:], RMS_EPS)
        self.setup_consts()
        self.h = self.alloc("h", [128, KC, T], F32)
        self.kT = [self.alloc("kT%d" % l, [128, KC, MEM], BF16) for l in range(2)]
        self.v = [self.alloc("v%d" % l, [128, 2, D], BF16) for l in range(2)]
        ph = self.phases
        dokv = "kv" in ph
        self.phase_boundary(None, 0, do_kv=dokv)
        for b in range(self.nseq):
            if "pool" in ph:
                self.phase_pool(b)
            if "attn0" in ph:
                self.phase_attn(b, 0, prefetch_moe=("moe0" in ph))
            if "moe0" in ph:
                self.phase_moe(b, 0, prefetched0=("attn0" in ph))
            if "rg" in ph:
                self.phase_rg(b)
            if "attn1" in ph:
                self.phase_attn(b, 1, prefetch_moe=("moe1" in ph))
            if "moe1" in ph:
                self.phase_moe(b, 1, prefetched0=("attn1" in ph))
            self.phase_boundary(b, b + 1 if b + 1 < self.nseq else None, do_norm=("final" in ph), do_kv=dokv)
        self.S.finish("sp")
        self.nwait = self.S.emit()


def host_layout(inp):
    f = lambda a: np.asarray(a, dtype=np.float32)
    bg = f(inp["rg_b_gate"])[0]
    rows = [f(inp["norm_mix"])[0], f(inp["norm_mix"])[1], f(inp["norm_xattn"])[0], f(inp["norm_xattn"])[1],
            f(inp["norm_mem"])[0], f(inp["norm_mem"])[1], f(inp["norm_moe"])[0], f(inp["norm_moe"])[1],
            f(inp["norm_final"]), f(inp["pool_scale"])[0],
            f(inp["rg_conv_w"])[0, 0], f(inp["rg_conv_w"])[0, 1], f(inp["rg_conv_w"])[0, 2], f(inp["rg_conv_w"])[0, 3],
            f(inp["rg_conv_b"])[0], f(inp["rg_lambda"])[0],
            bg[:, 0:256].reshape(D), bg[:, 256:512].reshape(D)]
    P = np.stack(rows)
    params = np.ascontiguousarray(P.reshape(NPAR, KC, 128).transpose(2, 1, 0)).reshape(128, KC * NPAR)
    wrl = []
    rbl = []
    for l in range(2):
        wg = f(inp["moe_wg"])[l]
        we = f(inp["moe_we"])[l].transpose(1, 0, 2).reshape(D, 16)
        w = np.concatenate([wg, we], axis=1)
        wrl.append(w.reshape(KC, 128, 20).transpose(1, 0, 2))
        rbl.append(np.concatenate([f(inp["moe_bg"])[l], f(inp["moe_be"])[l].reshape(16)]))
    wr = np.ascontiguousarray(np.stack(wrl, axis=1)).reshape(128, 2 * KC * 20)
    rbias = np.ascontiguousarray(np.stack(rbl, axis=1))
    return params, wr, rbias


_CACHE = {}


def kernel(**inputs):
    ncores = 8
    nseq = 2
    key = "full"
    if key not in _CACHE:
        _CACHE[key] = Builder(nseq=nseq, T=SEQ)
    bld = _CACHE[key]
    params, wr, rbias = host_layout(inputs)
    f = lambda a: np.ascontiguousarray(np.asarray(a, dtype=np.float32))
    x = f(inputs["x"])
    mem = f(inputs["mem"])
    shared = {
        "pool_w": f(inputs["pool_w"])[0], "rg_w_in": f(inputs["rg_w_in"])[0], "rg_w_gate": f(inputs["rg_w_gate"])[0],
        "rg_w_out": f(inputs["rg_w_out"])[0], "xa_wq": f(inputs["xa_wq"]), "xa_wkv": f(inputs["xa_wkv"]),
        "xa_wo": f(inputs["xa_wo"]), "moe_w_gu": f(inputs["moe_w_gu"]), "moe_w_down": f(inputs["moe_w_down"]),
        "params": params, "wr": wr, "rbias": rbias,
    }
    in_maps = []
    for c in range(ncores):
        d = dict(shared)
        d["x"] = x[c * nseq:(c + 1) * nseq]
        d["mem"] = mem[c * nseq:(c + 1) * nseq]
        in_maps.append(d)
    res = run_bass_kernel_spmd(bld.nc, in_maps, core_ids=list(range(ncores)))
    return np.concatenate([np.asarray(r["out"]) for r in res.results], axis=0).astype(np.float32)
```

```python
import numpy as np
import concourse.bass as bass
import concourse.mybir as mybir
from concourse.bass_utils import run_bass_kernel_spmd

F32 = mybir.dt.float32
BF16 = mybir.dt.bfloat16
F32R = mybir.dt.float32r
AF = mybir.ActivationFunctionType
ALU = mybir.AluOpType
AX = mybir.AxisListType

D = 1024
KC = 8
TT = 512
MEM = 256
NE = 16
DE = 512
SEQ = 2048
RMS_EPS = 1e-6
NPAR = 18
SB_START = 16640
SB_END = 229376


def _esz(dt):
    return 2 if dt == BF16 else 4


class Sched:
    NSEM_DMA = 40

    def __init__(self, nc):
        self.nc = nc
        self.eng = {"pe": nc.tensor, "act": nc.scalar, "dve": nc.vector, "pool": nc.gpsimd, "sp": nc.sync}
        self.ops = []
        self.reg = {}
        self.lastw = {}
        self.readers = {}
        self.waited = {e: {} for e in self.eng}
        self.stream_pos = {e: 0 for e in self.eng}
        self.dma_count = [0] * self.NSEM_DMA
        self.dma_rr = 0
        self.cache = {}

    def register(self, handle, space, base, esz, pstep, blk):
        self.reg[handle.name] = (space, base, esz, pstep, blk)

    def blocks_of(self, ap):
        info = self.reg.get(ap.tensor.name)
        if info is None:
            return ()
        key = (ap.tensor.name, int(ap.offset), tuple(ap.ap))
        r = self.cache.get(key)
        if r is not None:
            return r
        space, base, esz, pstep, blk = info
        off = int(ap.offset) % pstep
        dims = [(abs(s), n) for (s, n) in list(ap.ap)[1:] if n > 1 and s != 0]
        dims.sort(reverse=True)
        starts = [off]
        rest = []
        for (s, n) in dims:
            if s * esz >= blk and len(starts) * n <= 512:
                starts = [st + i * s for st in starts for i in range(n)]
            else:
                rest.append((s, n))
        ext = 1 + sum(s * (n - 1) for s, n in rest)
        out = set()
        for st in starts:
            b0 = (base + st * esz) // blk
            b1 = (base + (st + ext) * esz - 1) // blk
            for b in range(b0, b1 + 1):
                out.add((space, b))
        r = tuple(out)
        self.cache[key] = r
        return r

    def add(self, eng, fn, reads, writes, dma=False):
        idx = len(self.ops)
        op = {"eng": eng, "fn": fn, "waits": [], "marked": False, "dma": dma}
        if dma:
            si = self.dma_rr
            self.dma_rr = (self.dma_rr + 1) % self.NSEM_DMA
            prev = self.dma_count[si]
            self.dma_count[si] += 1
            op["stream"] = ("dma", si)
            op["pos"] = self.dma_count[si]
            if prev > 0:
                self._want(op, ("dma", si), prev, None)
        else:
            self.stream_pos[eng] += 1
            op["stream"] = eng
            op["pos"] = self.stream_pos[eng]
        deps = {}
        rb = set()
        for ap in reads:
            rb.update(self.blocks_of(ap))
        wb = set()
        for ap in writes:
            wb.update(self.blocks_of(ap))
        for b in rb:
            w = self.lastw.get(b)
            if w is not None:
                deps[w] = True
        for b in wb:
            w = self.lastw.get(b)
            if w is not None:
                deps[w] = True
            rd = self.readers.get(b)
            if rd:
                for d in rd.values():
                    deps[d] = True
        for d in deps:
            dop = self.ops[d]
            if dop["eng"] == "pe" and eng == "pe" and not dop["dma"] and not dma:
                continue
            self._want(op, dop["stream"], dop["pos"], dop)
        for b in rb:
            if b in wb:
                continue
            rd = self.readers.get(b)
            if rd is None:
                rd = {}
                self.readers[b] = rd
            rd[op["stream"]] = idx
        for b in wb:
            self.lastw[b] = idx
            self.readers[b] = {}
        self.ops.append(op)
        return idx

    def _want(self, op, stream, pos, dop):
        w = self.waited[op["eng"]]
        if w.get(stream, 0) >= pos:
            return
        w[stream] = pos
        op["waits"].append((stream, pos))
        if dop is not None:
            dop["marked"] = True

    def finish(self, final_eng="sp"):
        op = {"eng": final_eng, "fn": None, "waits": [], "marked": False, "dma": False, "stream": final_eng,
              "pos": self.stream_pos[final_eng] + 1}
        self.stream_pos[final_eng] += 1
        for si in range(self.NSEM_DMA):
            if self.dma_count[si] > 0:
                self._want(op, ("dma", si), self.dma_count[si], None)
        last = {}
        for o in self.ops:
            if not o["dma"]:
                last[o["stream"]] = o
        for st, o in last.items():
            if st != final_eng:
                self._want(op, st, o["pos"], o)
        self.ops.append(op)

    def emit(self):
        nc = self.nc
        sems = {e: nc.alloc_semaphore("s_" + e) for e in self.eng}
        dsems = [nc.alloc_semaphore("d_%d" % i) for i in range(self.NSEM_DMA)]
        counts = {e: {} for e in self.eng}
        run = {e: 0 for e in self.eng}
        for o in self.ops:
            if o["dma"]:
                continue
            if o["marked"]:
                run[o["stream"]] += 1
            counts[o["stream"]][o["pos"]] = run[o["stream"]]
        nwait = 0
        for o in self.ops:
            e = self.eng[o["eng"]]
            wl = []
            for (st, pos) in o["waits"]:
                if isinstance(st, tuple):
                    wl.append((dsems[st[1]], 16 * pos))
                else:
                    wl.append((sems[st], counts[st][pos]))
            nwait += len(wl)
            if o["fn"] is None:
                for (s, v) in wl:
                    e.wait_ge(s, v)
                continue
            if o["dma"]:
                for (s, v) in wl:
                    e.wait_ge(s, v)
                ins = o["fn"](e)
                ins.then_inc(dsems[o["stream"][1]], 16)
            else:
                attach = bool(wl)
                for (s, v) in (wl[:-1] if attach else wl):
                    e.wait_ge(s, v)
                ins = o["fn"](e)
                if attach:
                    ins._wait_ge(wl[-1][0], wl[-1][1])
                if o["marked"]:
                    ins.then_inc(sems[o["stream"]], 1)
        return nwait


class Builder:
    def __init__(self, nseq=2, T=SEQ, phases=("load", "kv", "pool", "attn0", "moe0", "rg", "attn1", "moe1", "final")):
        self.nseq = nseq
        self.T = T
        self.NT = T // TT
        self.phases = phases
        nc = bass.Bass("TRN2", target_bir_lowering=False)
        self.nc = nc
        self.S = Sched(nc)
        self.sb_ptr = SB_START
        self.bank_rr = 0
        dt = nc.dram_tensor
        self.x = dt("x", [nseq, T, D], F32, kind="ExternalInput").ap()
        self.mem = dt("mem", [nseq, MEM, D], F32, kind="ExternalInput").ap()
        self.pool_w = dt("pool_w", [4, 256, 256], F32, kind="ExternalInput").ap()
        self.rg_w_in = dt("rg_w_in", [D, 2 * D], F32, kind="ExternalInput").ap()
        self.rg_w_gate = dt("rg_w_gate", [4, 256, 512], F32, kind="ExternalInput").ap()
        self.rg_w_out = dt("rg_w_out", [D, D], F32, kind="ExternalInput").ap()
        self.xa_wq = dt("xa_wq", [2, D, D], F32, kind="ExternalInput").ap()
        self.xa_wkv = dt("xa_wkv", [2, D, 2 * D], F32, kind="ExternalInput").ap()
        self.xa_wo = dt("xa_wo", [2, D, D], F32, kind="ExternalInput").ap()
        self.moe_w_gu = dt("moe_w_gu", [2, NE, D, 2 * DE], F32, kind="ExternalInput").ap()
        self.moe_w_down = dt("moe_w_down", [2, NE, DE, D], F32, kind="ExternalInput").ap()
        self.params_d = dt("params", [128, KC * NPAR], F32, kind="ExternalInput").ap()
        self.wr_d = dt("wr", [128, 2 * KC * 20], F32, kind="ExternalInput").ap()
        self.rbias_d = dt("rbias", [20, 2], F32, kind="ExternalInput").ap()
        self.out = dt("out", [nseq, T, D], F32, kind="ExternalOutput").ap()
        self.ps = nc.alloc_psum_tensor("ps", [128, 8, 512], F32)
        self.S.register(self.ps, "P", 0, 4, 8 * 512, 512)
        self.build()

    def alloc(self, name, shape, dtype):
        esz = _esz(dtype)
        n = 1
        for s in shape[1:]:
            n *= s
        nbytes = n * esz
        off = self.sb_ptr
        self.sb_ptr = (off + nbytes + 255) // 256 * 256
        assert self.sb_ptr <= SB_END, "SBUF overflow at %s: %d" % (name, self.sb_ptr)
        if not hasattr(self, "offs"):
            self.offs = {}
        self.offs[name] = off
        self.uid = getattr(self, "uid", 0) + 1
        t = self.nc.alloc_sbuf_tensor_at("%s_%d" % (name, self.uid), list(shape), dtype, offset=off)
        self.S.register(t, "S", off, esz, n, 256)
        return t

    def alloc_at(self, name, shape, dtype, off):
        esz = _esz(dtype)
        n = 1
        for s_ in shape[1:]:
            n *= s_
        self.uid = getattr(self, "uid", 0) + 1
        t = self.nc.alloc_sbuf_tensor_at("%s_%d" % (name, self.uid), list(shape), dtype, offset=off)
        self.S.register(t, "S", off, esz, n, 256)
        return t

    def scan(self, out, d0, d1, init):
        self.S.add("dve", lambda e: e.tensor_tensor_scan(out=out, data0=d0, data1=d1, initial=init, op0=ALU.mult, op1=ALU.add),
                   [d0, d1, init], [out])

    def mark(self):
        return self.sb_ptr

    def release(self, m):
        self.sb_ptr = m

    def bank(self):
        k = self.bank_rr
        self.bank_rr = (k + 1) % 8
        return self.ps[:, k, :]

    def mm(self, out, lhsT, rhs, start, stop):
        self.S.add("pe", lambda e: e.matmul(out, lhsT, rhs, start=start, stop=stop), [lhsT, rhs], [out])

    def tr(self, out, in_, ident):
        self.S.add("pe", lambda e: e.transpose(out, in_, ident), [in_, ident], [out])

    def const_ap(self, val, npart=128):
        cc = getattr(self, "_consts", None)
        if cc is None:
            cc = self._consts = {}
        if val not in cc:
            t = self.alloc("c%d" % len(cc), [128, 1], F32)
            self.memset("pool", t[:], float(val))
            cc[val] = t
        return cc[val][0:npart, :]

    def act(self, out, in_, func, bias=None, scale=None):
        reads = [in_]
        kw = {}
        if isinstance(bias, (int, float)):
            bias = self.const_ap(float(bias), in_.shape[0])
        if bias is not None:
            kw["bias"] = bias
            if not isinstance(bias, (int, float)):
                reads.append(bias)
        if scale is not None:
            kw["scale"] = scale
            if not isinstance(scale, (int, float)):
                reads.append(scale)
        self.S.add("act", lambda e: e.activation(out=out, in_=in_, func=func, **kw), reads, [out])

    def copy(self, eng, out, in_):
        if eng == "act":
            self.act(out, in_, AF.Copy)
        else:
            self.S.add(eng, lambda e: e.tensor_copy(out=out, in_=in_), [in_], [out])

    def tt(self, eng, out, in0, in1, op):
        self.S.add(eng, lambda e: e.tensor_tensor(out=out, in0=in0, in1=in1, op=op), [in0, in1], [out])

    def ts(self, eng, out, in0, s1, s2, op0, op1=None):
        reads = [in0] + [s for s in (s1, s2) if s is not None and not isinstance(s, (int, float))]
        if op1 is None:
            self.S.add(eng, lambda e: e.tensor_scalar(out=out, in0=in0, scalar1=s1, scalar2=None, op0=op0), reads, [out])
        else:
            self.S.add(eng, lambda e: e.tensor_scalar(out=out, in0=in0, scalar1=s1, scalar2=s2, op0=op0, op1=op1), reads, [out])

    def stt(self, eng, out, in0, scalar, in1, op0, op1):
        reads = [in0, in1] + ([] if isinstance(scalar, (int, float)) else [scalar])
        self.S.add(eng, lambda e: e.scalar_tensor_tensor(out=out, in0=in0, scalar=scalar, in1=in1, op0=op0, op1=op1), reads, [out])

    def red(self, eng, out, in_, op):
        self.S.add(eng, lambda e: e.tensor_reduce(out=out, in_=in_, axis=AX.X, op=op), [in_], [out])

    def recip(self, out, in_):
        self.S.add("dve", lambda e: e.reciprocal(out=out, in_=in_), [in_], [out])

    def memset(self, eng, ap, val):
        self.S.add(eng, lambda e: e.memset(ap, val), [], [ap])

    def dma(self, q, out, in_):
        self.S.add(q, lambda e: e.dma_start(out=out, in_=in_), [in_], [out], dma=True)

    def setup_consts(self):
        nc = self.nc
        self.ident_f = self.alloc("ident_f", [128, 128], F32)
        self.ident_b = self.alloc("ident_b", [128, 128], BF16)
        self.ones_f = self.alloc("ones_f", [128, 128], F32)
        self.ones_b = self.alloc("ones_b", [128, 128], BF16)
        self.sel = self.alloc("sel", [16, NE, 128], F32R)
        self.params = self.alloc("params", [128, KC, NPAR], F32)
        self.wr = self.alloc("wr", [128, 2, KC, 20], F32)
        self.wrg = self.alloc("wrg", [128, 2, KC, 20], F32)
        self.rbias = self.alloc("rbias", [20, 2], F32)
        self.nsp = self.alloc("nsp", [128, KC], F32)
        self.nsp2 = self.alloc("nsp2", [128, KC], F32)
        self.ic = self.alloc("ic", [128, 16], F32)
        self.memset("pool", self.ident_f[:], 0.0)
        idf = self.ident_f
        self.S.add("pool", lambda e: e.affine_select(out=idf[:], in_=idf[:], pattern=[[-1, 128]], compare_op=ALU.not_equal,
                                                     fill=1.0, base=0, channel_multiplier=1), [idf[:]], [idf[:]])
        self.copy("dve", self.ident_b[:], self.ident_f[:])
        self.memset("dve", self.ones_f[:], 1.0)
        self.ones_r = self.alloc("ones_r", [128, 128], F32R)
        self.copy("act", self.ones_r[:], self.ones_f[:])
        self.memset("dve", self.ones_b[:], 1.0)
        self.copy("act", self.sel[:], self.ident_f[0:16, 0:16].unsqueeze(2).to_broadcast([16, NE, 128]))
        self.dma("sp", self.params[:], self.params_d.rearrange("p (c r) -> p c r", c=KC))
        self.dma("sp", self.wr[:], self.wr_d.rearrange("p (l c j) -> p l c j", l=2, c=KC))
        self.dma("sp", self.rbias[:], self.rbias_d)
        for l in range(2):
            for c in range(KC):
                self.ts("dve", self.wrg[:, l, c, :], self.wr[:, l, c, :], self.par(6 + l, c), None, ALU.mult)
        lam = self.params[:, :, 15]
        self.act(self.nsp[:], lam, AF.Exp, scale=-1.0)
        self.act(self.nsp[:], self.nsp[:], AF.Ln, bias=1.0)
        self.ts("dve", self.nsp2[:], self.nsp[:], -16.0, None, ALU.mult)
        self.nsph = self.alloc("nsph", [128, KC], F32)
        self.ts("dve", self.nsph[:], self.nsp[:], -4.0, None, ALU.mult)
        self.ts("dve", self.nsp[:], self.nsp[:], -8.0, None, ALU.mult)
        self.hb = self.alloc("hb", [128, KC, 2], F32)
        self.ts("dve", self.hb[:], self.params[:, :, 16:18], 0.5, None, ALU.mult)
        for t in range(16):
            self.memset("pool", self.ic[:, t:t + 1], 1.0 / (t + 1))

    def par(self, r, c):
        return self.params[:, c, r:r + 1]

    def alloc_norm_scratch(self, nsq=4, nrstd=2):
        self.sq = [self.alloc("sq%d" % i, [128, TT], F32R) for i in range(nsq)]
        self.std = self.alloc("std", [128, TT], F32)
        self.rstd = [self.alloc("rstd%d" % i, [128, TT], F32) for i in range(nrstd)]
        self.norm_rr = 0

    def norm_stats(self, chunks, N):
        bank = self.bank()
        for c in range(KC):
            sq = self.sq[c % len(self.sq)]
            self.act(sq[:, 0:N], chunks[c], AF.Square)
            self.mm(bank[:, 0:N], self.ones_r[:], sq[:, 0:N], start=(c == 0), stop=(c == KC - 1))
        self.act(self.std[:, 0:N], bank[:, 0:N], AF.Ln, bias=self.eps_ap, scale=1.0 / D)
        r = self.rstd[self.norm_rr % len(self.rstd)]
        self.norm_rr += 1
        self.act(r[:, 0:N], self.std[:, 0:N], AF.Exp, scale=-0.5)
        return r[:, 0:N]

    def norm_apply(self, chunks, rstd, grow, outs, eng="dve"):
        for c in range(KC):
            self.stt(eng, outs[c], chunks[c], self.par(grow, c), rstd, ALU.mult, ALU.mult)

    def h_chunks(self, tt):
        return [self.h[:, c, tt * TT:(tt + 1) * TT] for c in range(KC)]

    def phase_load(self, b):
        m = self.mark()
        xs = [self.alloc("xs%d" % i, [128, D], F32) for i in range(2)]
        for i in range(self.T // 128):
            xsb = xs[i % 2]
            self.dma("sp", xsb[:], self.x[b, i * 128:(i + 1) * 128, :])
            for half in range(2):
                bank = self.bank()
                for cc in range(4):
                    c = half * 4 + cc
                    self.tr(bank[:, cc * 128:(cc + 1) * 128], xsb[:, c * 128:(c + 1) * 128], self.ident_f[:])
                self.copy("act" if half == 0 else "dve", self.h[:, half * 4:half * 4 + 4, i * 128:(i + 1) * 128],
                          bank.rearrange("p (c t) -> p c t", c=4))
        self.release(m)

    def phase_final(self, b, do_norm=True):
        m = self.mark()
        self.alloc_norm_scratch()
        hn = self.alloc("hn", [128, KC, TT], F32)
        ot = [self.alloc("ot%d" % i, [128, D], F32) for i in range(2)]
        k = 0
        for tt in range(self.NT):
            hc = self.h_chunks(tt)
            if do_norm:
                rstd = self.norm_stats(hc, TT)
                self.norm_apply(hc, rstd, 8, [hn[:, c, :] for c in range(KC)])
                src = [hn[:, c, :] for c in range(KC)]
            else:
                src = hc
            for s in range(4):
                o = ot[k % 2]
                k += 1
                for half in range(2):
                    bank = self.bank()
                    for cc in range(4):
                        c = half * 4 + cc
                        self.tr(bank[:, cc * 128:(cc + 1) * 128], src[c][:, s * 128:(s + 1) * 128], self.ident_f[:])
                    self.copy("act" if half == 0 else "dve", o[:, half * 512:(half + 1) * 512], bank)
                t0 = tt * TT + s * 128
                self.dma("sp", self.out[b, t0:t0 + 128, :], o[:])
        self.release(m)

    def load_w(self, dst, src, kchunks, ncols, split_cols=1024):
        sv = src.rearrange("(k p) n -> p k n", p=128)
        for c0 in range(0, ncols, split_cols):
            c1 = min(ncols, c0 + split_cols)
            self.dma("pool", dst[:, :, c0:c1], sv[:, :, c0:c1])

    def phase_kv(self, b):
        m = self.mark()
        self.alloc_norm_scratch()
        wkv = self.alloc("wkv", [128, KC, 2 * D], BF16)
        ms = [self.alloc("ms%d" % i, [128, D], F32) for i in range(2)]
        memT = self.alloc("memT", [128, KC, MEM], F32)
        memn = self.alloc("memn", [128, KC, MEM], BF16)
        for i in range(2):
            self.dma("sp", ms[i][:], self.mem[b, i * 128:(i + 1) * 128, :])
            for half in range(2):
                bank = self.bank()
                for cc in range(4):
                    c = half * 4 + cc
                    self.tr(bank[:, cc * 128:(cc + 1) * 128], ms[i][:, c * 128:(c + 1) * 128], self.ident_f[:])
                self.copy("act" if half == 0 else "dve", memT[:, half * 4:half * 4 + 4, i * 128:(i + 1) * 128],
                          bank.rearrange("p (c t) -> p c t", c=4))
        for l in range(2):
            self.load_w(wkv, self.xa_wkv[l], KC, 2 * D)
            mc_ = [memT[:, c, :] for c in range(KC)]
            rstd = self.norm_stats(mc_, MEM)
            self.norm_apply(mc_, rstd, 4 + l, [memn[:, c, :] for c in range(KC)])
            for o in range(KC):
                bank = self.bank()
                for c in range(KC):
                    self.mm(bank[:, 0:MEM], wkv[:, c, o * 128:(o + 1) * 128], memn[:, c, :], start=(c == 0), stop=(c == KC - 1))
                self.copy("act" if o % 2 == 0 else "dve", self.kT[l][:, o, :], bank[:, 0:MEM])
            for mc in range(2):
                for n in range(2):
                    bank = self.bank()
                    for c in range(KC):
                        self.mm(bank, memn[:, c, mc * 128:(mc + 1) * 128], wkv[:, c, D + n * 512:D + (n + 1) * 512],
                                start=(c == 0), stop=(c == KC - 1))
                    self.copy("act" if n % 2 == 0 else "dve", self.v[l][:, mc, n * 512:(n + 1) * 512], bank)
        self.release(m)

    def phase_boundary(self, b_fin, b_next, do_norm=True, do_kv=True):
        m = self.mark()
        self.alloc_norm_scratch()
        pieces_f, pieces_k, pieces_l = [], [], []
        if b_fin is not None:
            hn = self.alloc("hn", [128, KC, TT], F32)
            ot = [self.alloc("ot%d" % i, [128, D], F32) for i in range(2)]
            cnt = [0]

            def fin_tile(tt, b=b_fin):
                hc = self.h_chunks(tt)
                if do_norm:
                    rstd = self.norm_stats(hc, TT)
                    self.norm_apply(hc, rstd, 8, [hn[:, c, :] for c in range(KC)])
                    src = [hn[:, c, :] for c in range(KC)]
                else:
                    src = hc
                for s_ in range(4):
                    o = ot[cnt[0] % 2]
                    cnt[0] += 1
                    for half in range(2):
                        bank = self.bank()
                        for cc in range(4):
                            c = half * 4 + cc
                            self.tr(bank[:, cc * 128:(cc + 1) * 128], src[c][:, s_ * 128:(s_ + 1) * 128], self.ident_f[:])
                        self.copy("act" if half == 0 else "dve", o[:, half * 512:(half + 1) * 512], bank)
                    t0 = tt * TT + s_ * 128
                    self.dma("sp", self.out[b, t0:t0 + 128, :], o[:])

            pieces_f = [(lambda tt=tt: fin_tile(tt)) for tt in range(self.NT)]
        if b_next is not None:
            xs = [self.alloc("xs%d" % i, [128, D], F32) for i in range(2)]

            def load_tiles(i0, i1, b=b_next):
                for i in range(i0, i1):
                    xsb = xs[i % 2]
                    self.dma("sp", xsb[:], self.x[b, i * 128:(i + 1) * 128, :])
                    for half in range(2):
                        bank = self.bank()
                        for cc in range(4):
                            c = half * 4 + cc
                            self.tr(bank[:, cc * 128:(cc + 1) * 128], xsb[:, c * 128:(c + 1) * 128], self.ident_f[:])
                        self.copy("act" if half == 0 else "dve", self.h[:, half * 4:half * 4 + 4, i * 128:(i + 1) * 128],
                                  bank.rearrange("p (c t) -> p c t", c=4))

            n128 = self.T // 128
            q4 = max(1, n128 // 4)
            pieces_l = [(lambda i0=i0: load_tiles(i0, min(n128, i0 + q4))) for i0 in range(0, n128, q4)]
            if do_kv:
                wkv = self.alloc("wkv", [128, KC, 2 * D], BF16)
                ms = [self.alloc("ms%d" % i, [128, D], F32) for i in range(2)]
                memT = self.alloc("memT", [128, KC, MEM], F32)
                memn = self.alloc("memn", [128, KC, MEM], BF16)
                bn = b_next

                def k0():
                    for i in range(2):
                        self.dma("sp", ms[i][:], self.mem[bn, i * 128:(i + 1) * 128, :])
                    self.load_w(wkv, self.xa_wkv[0], KC, 2 * D)

                def k1():
                    for i in range(2):
                        for half in range(2):
                            bank = self.bank()
                            for cc in range(4):
                                c = half * 4 + cc
                                self.tr(bank[:, cc * 128:(cc + 1) * 128], ms[i][:, c * 128:(c + 1) * 128], self.ident_f[:])
                            self.copy("act" if half == 0 else "dve", memT[:, half * 4:half * 4 + 4, i * 128:(i + 1) * 128],
                                      bank.rearrange("p (c t) -> p c t", c=4))

                def kk(l):
                    mc_ = [memT[:, c, :] for c in range(KC)]
                    rstd = self.norm_stats(mc_, MEM)
                    self.norm_apply(mc_, rstd, 4 + l, [memn[:, c, :] for c in range(KC)])
                    for o in range(KC):
                        bank = self.bank()
                        for c in range(KC):
                            self.mm(bank[:, 0:MEM], wkv[:, c, o * 128:(o + 1) * 128], memn[:, c, :], start=(c == 0), stop=(c == KC - 1))
                        self.copy("act" if o % 2 == 0 else "dve", self.kT[l][:, o, :], bank[:, 0:MEM])

                def kv_(l):
                    for mc in range(2):
                        for n in range(2):
                            bank = self.bank()
                            for c in range(KC):
                                self.mm(bank, memn[:, c, mc * 128:(mc + 1) * 128], wkv[:, c, D + n * 512:D + (n + 1) * 512],
                                        start=(c == 0), stop=(c == KC - 1))
                            self.copy("act" if n % 2 == 0 else "dve", self.v[l][:, mc, n * 512:(n + 1) * 512], bank)
                    if l == 0:
                        self.load_w(wkv, self.xa_wkv[1], KC, 2 * D)

                pieces_k = [k0, k1, lambda: kk(0), lambda: kv_(0), lambda: kk(1), lambda: kv_(1)]
        order = []
        pk = list(pieces_k)
        pf = list(pieces_f)
        pl = list(pieces_l)
        if pk:
            order.append(pk.pop(0))
        while pf or pk or pl:
            if pf:
                order.append(pf.pop(0))
            if pk:
                order.append(pk.pop(0))
            if not pf and pl:
                order.append(pl.pop(0))
        for fn in order:
            fn()
        self.release(m)

    def phase_attn(self, b, l, prefetch_moe=False):
        m = self.mark()
        if prefetch_moe:
            pwgu, pwdn = self.moe_slots(1)
        self.alloc_norm_scratch(nsq=2)
        wq = self.alloc("wq", [128, KC, D], BF16)
        wo = self.alloc("wo", [128, KC, D], BF16)
        self.load_w(wq, self.xa_wq[l], KC, D)
        self.load_w(wo, self.xa_wo[l], KC, D)
        if prefetch_moe:
            self.load_expert(l, 0, pwgu, pwdn)
        xn = [self.alloc("xn_t%d" % i, [128, KC, TT], BF16) for i in range(2)]
        qT = [self.alloc("qT%d" % i, [128, KC, TT], BF16) for i in range(2)]
        oT = [self.alloc("oT%d" % i, [128, KC, TT], BF16) for i in range(1)]
        E = [self.alloc("E%d" % i, [128, 2, TT], BF16) for i in range(2)]
        rden = [self.alloc("rden%d" % i, [128, TT], F32) for i in range(1)]
        kT, v = self.kT[l], self.v[l]

        def N(tt):
            hc = self.h_chunks(tt)
            rstd = self.norm_stats(hc, TT)
            self.norm_apply(hc, rstd, 2 + l, [xn[tt % 2][:, c, :] for c in range(KC)])

        def Q(tt):
            x_, q_ = xn[tt % 2], qT[tt % 2]
            for o in range(KC):
                bank = self.bank()
                for c in range(KC):
                    self.mm(bank, wq[:, c, o * 128:(o + 1) * 128], x_[:, c, :], start=(c == 0), stop=(c == KC - 1))
                self.act(q_[:, o, :], bank, AF.Copy, scale=1.0 / 16.0)

        def H(tt):
            q_, o_ = qT[tt % 2], oT[0]

            def scores(hd):
                e_ = E[hd % 2]
                for mc in range(2):
                    bank = self.bank()
                    for dc in range(2):
                        self.mm(bank, kT[:, 2 * hd + dc, mc * 128:(mc + 1) * 128], q_[:, 2 * hd + dc, :],
                                start=(dc == 0), stop=(dc == 1))
                    self.act(e_[:, mc, :], bank, AF.Exp)

            def pv(hd):
                e_ = E[hd % 2]
                r_ = rden[0]
                bank = self.bank()
                for mc in range(2):
                    self.mm(bank, self.ones_b[:], e_[:, mc, :], start=(mc == 0), stop=(mc == 1))
                self.act(r_[:], bank, AF.Ln)
                self.act(r_[:], r_[:], AF.Exp, scale=-1.0)
                for dc in range(2):
                    bank = self.bank()
                    for mc in range(2):
                        self.mm(bank, v[:, mc, (2 * hd + dc) * 128:(2 * hd + dc + 1) * 128], e_[:, mc, :],
                                start=(mc == 0), stop=(mc == 1))
                    self.tt("dve", o_[:, 2 * hd + dc, :], bank, r_[:], ALU.mult)

            scores(0)
            for hd in range(4):
                if hd + 1 < 4:
                    scores(hd + 1)
                pv(hd)

        def O(tt):
            hc = self.h_chunks(tt)
            o_ = oT[0]
            for o in range(KC):
                bank = self.bank()
                for c in range(KC):
                    self.mm(bank, wo[:, c, o * 128:(o + 1) * 128], o_[:, c, :], start=(c == 0), stop=(c == KC - 1))
                self.tt("dve", hc[o], hc[o], bank, ALU.add)

        N(0)
        Q(0)
        for tt in range(self.NT):
            if tt + 1 < self.NT:
                N(tt + 1)
            H(tt)
            if tt + 1 < self.NT:
                Q(tt + 1)
            O(tt)
        self.release(m)

    def phase_pool(self, b):
        m = self.mark()
        self.alloc_norm_scratch()
        W = 16 + TT
        pw = self.alloc("pw", [128, 4, 2, 256], BF16)
        self.dma("pool", pw[:], self.pool_w.rearrange("g (kc p) o -> p g kc o", p=128))
        xnfs = [self.alloc("xnf%d" % i, [128, KC, W], F32) for i in range(2)]
        lv = [[self.alloc("lv%d%d" % (g, k), [128, 2, W], F32) for k in range(g + 1)] for g in range(4)]
        pb = self.alloc("pb", [128, KC, TT], BF16)
        fix = [self.alloc("fix%d" % g, [128, 2, 16], F32) for g in range(4)]
        pend = [None]
        for tt in range(self.NT):
            hc = self.h_chunks(tt)
            xnf = xnfs[tt % 2]
            if tt == 0:
                self.memset("dve", xnf[:, :, 0:16], 0.0)
            else:
                self.copy("act", xnf[:, :, 0:16], xnfs[(tt - 1) % 2][:, :, TT:TT + 16])
            rstd = self.norm_stats(hc, TT)
            xgs = [xnf[:, 2 * g:2 * g + 2, :] for g in range(4)]

            def prep(g):
                xg = xgs[g]
                for cc in range(2):
                    c = 2 * g + cc
                    self.stt("dve", xnf[:, c, 16:W], hc[c], self.par(0, c), rstd, ALU.mult, ALU.mult)

            def levels(g, eng):
                src = xgs[g]
                lo = 0
                for k in range(g + 1):
                    sh = 1 << k
                    nlo = lo + sh
                    dst = lv[g][k]
                    self.tt(eng, dst[:, :, nlo:W], src[:, :, nlo:W], src[:, :, lo:W - sh], ALU.add)
                    src = dst
                    lo = nlo
                return src

            def finish(g, src):
                xg = xgs[g]
                w = 2 << g
                self.stt("dve", pb[:, 2 * g:2 * g + 2, :], src[:, :, 16:W], 1.0 / w, xg[:, :, 16:W], ALU.mult, ALU.subtract)
                if tt == 0:
                    nf = w - 1
                    self.tt("dve", fix[g][:, :, 0:nf], src[:, :, 16:16 + nf],
                            self.ic[:, 0:nf].unsqueeze(1).to_broadcast([128, 2, nf]), ALU.mult)
                    self.tt("dve", pb[:, 2 * g:2 * g + 2, 0:nf], fix[g][:, :, 0:nf], xg[:, :, 16:16 + nf], ALU.subtract)
                banks = []
                for oc in range(2):
                    bank = self.bank()
                    for kc in range(2):
                        self.mm(bank, pw[:, g, kc, oc * 128:(oc + 1) * 128], pb[:, 2 * g + kc, :], start=(kc == 0), stop=(kc == 1))
                    banks.append(bank)
                return banks

            def update(g, banks, hc_):
                for oc in range(2):
                    c = 2 * g + oc
                    self.stt("dve", hc_[c], banks[oc], self.par(9, c), hc_[c], ALU.mult, ALU.add)

            for g in range(4):
                prep(g)
                bk = finish(g, levels(g, "dve"))
                if pend[0] is not None:
                    update(*pend[0])
                pend[0] = (g, bk, hc)
        update(*pend[0])
        self.release(m)

    def phase_rg(self, b):
        m = self.mark()
        self.alloc_norm_scratch(nsq=2)
        w_in = self.alloc("w_in", [128, KC, 2 * D], BF16)
        w_gate = self.alloc("w_gate", [128, 4, 2, 512], BF16)
        w_out = self.alloc("w_out", [128, KC, D], BF16)
        self.load_w(w_in, self.rg_w_in, KC, 2 * D)
        self.dma("pool", w_gate[:], self.rg_w_gate.rearrange("h (kc p) o -> p h kc o", p=128))
        self.load_w(w_out, self.rg_w_out, KC, D)
        xbh = self.alloc("xbh", [128, KC, 4], F32)
        st = self.alloc("st", [128, KC], F32)
        self.memset("pool", xbh[:], 0.0)
        self.memset("pool", st[:], 0.0)
        xns = [self.alloc("xn_t", [128, KC, TT], BF16), self.alloc_at("xn_t2", [128, KC, TT], BF16, self.offs["sel"])]
        y_t = self.alloc("y_t", [128, KC, TT], BF16)
        xbp = [[self.alloc("xbp%d%d" % (i, j), [128, 3 + TT], F32) for j in range(2)] for i in range(2)]
        xc = [[self.alloc("xc%d%d" % (i, j), [128, TT], F32) for j in range(2)] for i in range(2)]
        xcb_all = self.alloc_at("xcb", [128, 2, 2, TT], BF16, self.offs["kT0"])
        R = [self.alloc("R%d" % i, [128, TT], F32) for i in range(2)]
        I = [self.alloc("I%d" % i, [128, TT], F32) for i in range(2)]
        A = [self.alloc("A%d" % i, [128, TT], F32) for i in range(2)]

        cur = {"xn": xns[0]}

        def stageA(hd):
            xn = cur["xn"]
            hp = hd % 2
            for cc in range(2):
                c = 2 * hd + cc
                bank = self.bank()
                for k in range(KC):
                    self.mm(bank, w_in[:, k, c * 128:(c + 1) * 128], xn[:, k, :], start=(k == 0), stop=(k == KC - 1))
                xb = xbp[hp][cc]
                self.copy("pool", xb[:, 0:3], xbh[:, c, 0:3])
                self.copy("dve", xb[:, 3:3 + TT], bank)
                x_ = xc[hp][cc]
                self.ts("dve", x_[:], xb[:, 3:3 + TT], self.par(13, c), self.par(14, c), ALU.mult, ALU.add)
                self.copy("pool", xbh[:, c, 0:3], xb[:, TT:TT + 3])
                for k in range(3):
                    self.stt("dve", x_[:], xb[:, k:k + TT], self.par(10 + k, c), x_[:], ALU.mult, ALU.add)
                self.copy("dve", xcb_all[:, hp, cc, :], x_[:])

        def stageB(hd):
            xn = cur["xn"]
            hp = hd % 2
            gb = []
            for oc in range(4):
                bank = self.bank()
                for kc in range(2):
                    self.mm(bank, w_gate[:, hd, kc, oc * 128:(oc + 1) * 128], xcb_all[:, hp, kc, :], start=(kc == 0), stop=(kc == 1))
                gb.append(bank)
            yb = []
            for cc in range(2):
                c = 2 * hd + cc
                bank = self.bank()
                for k in range(KC):
                    self.mm(bank, w_in[:, k, D + c * 128:D + (c + 1) * 128], xn[:, k, :], start=(k == 0), stop=(k == KC - 1))
                yb.append(bank)
            for cc in range(2):
                c = 2 * hd + cc
                self.act(R[cc][:], gb[cc], AF.Sigmoid, bias=self.par(16, c))
                self.act(I[cc][:], gb[2 + cc], AF.Sigmoid, bias=self.par(17, c))
            for cc in range(2):
                self.tt("dve", I[cc][:], I[cc][:], xc[hp][cc][:], ALU.mult)
            for cc in range(2):
                c = 2 * hd + cc
                self.act(A[cc][:], R[cc][:], AF.Exp, scale=self.nsp[:, c:c + 1])
                self.act(R[cc][:], R[cc][:], AF.Exp, scale=self.nsp2[:, c:c + 1])
            for cc in range(2):
                self.act(R[cc][:], R[cc][:], AF.Sqrt, bias=1.0, scale=-1.0)
            for cc in range(2):
                c = 2 * hd + cc
                self.tt("dve", I[cc][:], I[cc][:], R[cc][:], ALU.mult)
                stc = st[:, c:c + 1]
                self.scan(R[cc][:], A[cc][:], I[cc][:], stc)
                self.copy("dve", stc, R[cc][:, TT - 1:TT])
            for cc in range(2):
                self.act(xc[hp][cc][:], yb[cc], AF.Gelu_apprx_tanh)
            for cc in range(2):
                c = 2 * hd + cc
                self.tt("dve", y_t[:, c, :], R[cc][:], xc[hp][cc][:], ALU.mult)

        def NRM(tt):
            hc_ = self.h_chunks(tt)
            rstd = self.norm_stats(hc_, TT)
            self.norm_apply(hc_, rstd, 1, [xns[tt % 2][:, c, :] for c in range(KC)])

        NRM(0)
        cur["xn"] = xns[0]
        stageA(0)
        for tt in range(self.NT):
            hc = self.h_chunks(tt)
            for hd in range(4):
                cur["xn"] = xns[tt % 2]
                if hd + 1 < 4:
                    stageA(hd + 1)
                if hd == 3 and tt + 1 < self.NT:
                    NRM(tt + 1)
                stageB(hd)
            if tt + 1 < self.NT:
                cur["xn"] = xns[(tt + 1) % 2]
                stageA(0)
            for o in range(KC):
                bank = self.bank()
                for c in range(KC):
                    self.mm(bank, w_out[:, c, o * 128:(o + 1) * 128], y_t[:, c, :], start=(c == 0), stop=(c == KC - 1))
                self.tt("dve", hc[o], hc[o], bank, ALU.add)
        self.release(m)

    def moe_slots(self, n=2):
        wgu, wdn = [], []
        for i in range(n):
            wgu.append(self.alloc("wgu%d" % i, [128, KC, 2 * DE], BF16))
            wdn.append(self.alloc("wdn%d" % i, [128, 4, D], BF16))
        return wgu, wdn

    def load_expert(self, l, e, wgu, wdn):
        s = e % 2
        self.load_w(wgu[s], self.moe_w_gu[l, e], KC, 2 * DE)
        self.load_w(wdn[s], self.moe_w_down[l, e], 4, D)

    def phase_moe(self, b, l, prefetched0=False):
        m = self.mark()
        T, NT = self.T, self.NT
        wgu, wdn = self.moe_slots()
        xn = self.alloc("xn_full", [128, KC, T], BF16)
        cT = self.alloc("cT", [16, T], F32R)
        if not prefetched0:
            self.load_expert(l, 0, wgu, wdn)
        self.load_expert(l, 1, wgu, wdn)
        self.copy("act", self.sel[:], self.ident_f[0:16, 0:16].unsqueeze(2).to_broadcast([16, NE, 128]))
        self.alloc_norm_scratch(nsq=2, nrstd=1)
        lt = self.alloc("lt", [20, TT], F32)
        Ls = self.alloc("Ls", [128, 4, 20], F32)
        sm = {}
        for nm, shp in (("gmax", [128, 4]), ("gsh", [128, 4, 4]), ("gsum", [128, 4]), ("gp", [128, 4]), ("gm", [128, 4, 4]),
                        ("m1", [128, 4, 4]), ("esh", [128, 4, 4, 4]), ("mask1", [128, 4, 4, 4]), ("m2", [128, 4, 4]),
                        ("top2", [128, 4, 4, 4]), ("eex", [128, 4, 4, 4]), ("den", [128, 4, 4]), ("comb", [128, 4, 16])):
            sm[nm] = self.alloc(nm, shp, F32)
        sg = [self.alloc("sg%d" % i, [128, TT], F32) for i in range(2)]
        t1 = [self.alloc("t1%d" % i, [128, TT], F32) for i in range(1)]
        cb = [self.alloc("cb%d" % i, [128, TT], F32) for i in range(2)]
        hh = [self.alloc_at("hh0", [128, 4, TT], BF16, self.offs["kT0"]),
              self.alloc_at("hh1", [128, 4, TT], BF16, self.offs["v0"])]
        state = {}

        def NRa(tt):
            hc = self.h_chunks(tt)
            rstd = self.norm_stats(hc, TT)
            self.norm_apply(hc, rstd, 6 + l, [xn[:, c, tt * TT:(tt + 1) * TT] for c in range(KC)])
            bank = self.bank()
            for c in range(KC):
                self.mm(bank[0:20, :], self.wrg[:, l, c, :], hc[c], start=(c == 0), stop=(c == KC - 1))
            self.tt("dve", lt[:], bank[0:20, :], rstd[0:20, :], ALU.mult)
            self.act(lt[:], lt[:], AF.Identity, bias=self.rbias[:, l:l + 1])

        def NRb(tt):
            bankL = self.bank()
            for s in range(4):
                self.tr(bankL[:, s * 20:(s + 1) * 20], lt[0:20, s * 128:(s + 1) * 128], self.ident_f[0:20, 0:20])
            self.copy("act", Ls[:], bankL[:, 0:80].rearrange("p (s j) -> p s j", s=4))
            gl = Ls[:, :, 0:4]
            el = Ls[:, :, 4:20].rearrange("p s (g e) -> p s g e", g=4)
            bc3 = lambda ap: ap.unsqueeze(2).to_broadcast([128, 4, 4])
            bc4 = lambda ap: ap.unsqueeze(3).to_broadcast([128, 4, 4, 4])
            self.red("dve", sm["gmax"][:], gl, ALU.max)
            self.tt("dve", sm["gsh"][:], gl, bc3(sm["gmax"][:]), ALU.subtract)
            self.ts("dve", sm["gm"][:], sm["gsh"][:], 0.0, None, ALU.is_ge)
            self.act(sm["gsh"][:], sm["gsh"][:], AF.Exp)
            self.red("dve", sm["gsum"][:], sm["gsh"][:], ALU.add)
            self.recip(sm["gp"][:], sm["gsum"][:])
            self.tt("dve", sm["gm"][:], sm["gm"][:], bc3(sm["gp"][:]), ALU.mult)
            self.red("dve", sm["m1"][:], el, ALU.max)
            self.tt("dve", sm["esh"][:], el, bc4(sm["m1"][:]), ALU.subtract)
            self.ts("dve", sm["mask1"][:], sm["esh"][:], 0.0, None, ALU.is_ge)
            self.stt("dve", sm["mask1"][:], sm["mask1"][:], -1e30, sm["esh"][:], ALU.mult, ALU.add)
            self.red("dve", sm["m2"][:], sm["mask1"][:], ALU.max)
            self.tt("dve", sm["top2"][:], sm["esh"][:], bc4(sm["m2"][:]), ALU.is_ge)
            self.act(sm["eex"][:], sm["esh"][:], AF.Exp)
            self.tt("dve", sm["eex"][:], sm["eex"][:], sm["top2"][:], ALU.mult)
            self.red("dve", sm["den"][:], sm["eex"][:], ALU.add)
            self.recip(sm["den"][:], sm["den"][:])
            self.tt("dve", sm["den"][:], sm["den"][:], sm["gm"][:], ALU.mult)
            self.tt("dve", sm["comb"][:].rearrange("p s (g e) -> p s g e", g=4), sm["eex"][:], bc4(sm["den"][:]), ALU.mult)

        def NRc(tt):
            bankC = self.bank()
            for s in range(4):
                self.tr(bankC[0:16, s * 128:(s + 1) * 128], sm["comb"][:, s, :], self.ident_f[:])
            self.copy("act", cT[:, tt * TT:(tt + 1) * TT], bankC[0:16, :])

        seq = [(e, tt) for e in range(NE) for tt in range(NT)]

        def gu(q):
            e, tt = seq[q]
            s = e % 2
            par = q % 2
            bank = self.bank()
            self.mm(bank, self.sel[:, e, :], cT[:, tt * TT:(tt + 1) * TT], start=True, stop=True)
            self.copy("act", cb[par][:], bank)
            for j in range(4):
                bg = self.bank()
                for k in range(KC):
                    self.mm(bg, wgu[s][:, k, j * 128:(j + 1) * 128], xn[:, k, tt * TT:(tt + 1) * TT], start=(k == 0), stop=(k == KC - 1))
                bu = self.bank()
                for k in range(KC):
                    self.mm(bu, wgu[s][:, k, DE + j * 128:DE + (j + 1) * 128], xn[:, k, tt * TT:(tt + 1) * TT], start=(k == 0), stop=(k == KC - 1))
                self.act(sg[j % 2][:], bg, AF.Silu)
                self.tt("dve", t1[0][:], sg[j % 2][:], bu, ALU.mult)
                self.tt("dve", hh[par][:, j, :], t1[0][:], cb[par][:], ALU.mult)

        def down(q):
            e, tt = seq[q]
            s = e % 2
            par = q % 2
            hc = self.h_chunks(tt)
            for mo in range(KC):
                bank = self.bank()
                for j in range(4):
                    self.mm(bank, wdn[s][:, j, mo * 128:(mo + 1) * 128], hh[par][:, j, :], start=(j == 0), stop=(j == 3))
                self.tt("dve", hc[mo], hc[mo], bank, ALU.add)
            if tt == NT - 1 and e + 2 < NE:
                self.load_expert(l, e + 2, wgu, wdn)

        NRa(0); NRb(0); NRc(0)
        for q in range(len(seq)):
            e, tt = seq[q]
            nxt = tt + 1
            if e == 0 and nxt < NT:
                NRa(nxt)
            gu(q)
            if e == 0 and nxt < NT:
                NRb(nxt)
            if q > 0:
                down(q - 1)
            if e == 0 and nxt < NT:
                NRc(nxt)
        down(len(seq) - 1)
        self.release(m)

    def build(self):
        T = self.T
        self.eps_t = self.alloc("eps", [128, 1], F32)
        self.eps_ap = self.eps_t[:]
        self.memset("dve", self.eps_t[:], RMS_EPS)
        self.setup_consts()
        self.h = self.alloc("h", [128, KC, T], F32)
        self.kT = [self.alloc("kT%d" % l, [128, KC, MEM], BF16) for l in range(2)]
        self.v = [self.alloc("v%d" % l, [128, 2, D], BF16) for l in range(2)]
        ph = self.phases
        dokv = "kv" in ph
        self.phase_boundary(None, 0, do_kv=dokv)
        for b in range(self.nseq):
            if "pool" in ph:
                self.phase_pool(b)
            if "attn0" in ph:
                self.phase_attn(b, 0, prefetch_moe=("moe0" in ph))
            if "moe0" in ph:
                self.phase_moe(b, 0, prefetched0=("attn0" in ph))
            if "rg" in ph:
                self.phase_rg(b)
            if "attn1" in ph:
                self.phase_attn(b, 1, prefetch_moe=("moe1" in ph))
            if "moe1" in ph:
                self.phase_moe(b, 1, prefetched0=("attn1" in ph))
            self.phase_boundary(b, b + 1 if b + 1 < self.nseq else None, do_norm=("final" in ph), do_kv=dokv)
        self.S.finish("sp")
        self.nwait = self.S.emit()


def host_layout(inp):
    f = lambda a: np.asarray(a, dtype=np.float32)
    bg = f(inp["rg_b_gate"])[0]
    rows = [f(inp["norm_mix"])[0], f(inp["norm_mix"])[1], f(inp["norm_xattn"])[0], f(inp["norm_xattn"])[1],
            f(inp["norm_mem"])[0], f(inp["norm_mem"])[1], f(inp["norm_moe"])[0], f(inp["norm_moe"])[1],
            f(inp["norm_final"]), f(inp["pool_scale"])[0],
            f(inp["rg_conv_w"])[0, 0], f(inp["rg_conv_w"])[0, 1], f(inp["rg_conv_w"])[0, 2], f(inp["rg_conv_w"])[0, 3],
            f(inp["rg_conv_b"])[0], f(inp["rg_lambda"])[0],
            bg[:, 0:256].reshape(D), bg[:, 256:512].reshape(D)]
    P = np.stack(rows)
    params = np.ascontiguousarray(P.reshape(NPAR, KC, 128).transpose(2, 1, 0)).reshape(128, KC * NPAR)
    wrl = []
    rbl = []
    for l in range(2):
        wg = f(inp["moe_wg"])[l]
        we = f(inp["moe_we"])[l].transpose(1, 0, 2).reshape(D, 16)
        w = np.concatenate([wg, we], axis=1)
        wrl.append(w.reshape(KC, 128, 20).transpose(1, 0, 2))
        rbl.append(np.concatenate([f(inp["moe_bg"])[l], f(inp["moe_be"])[l].reshape(16)]))
    wr = np.ascontiguousarray(np.stack(wrl, axis=1)).reshape(128, 2 * KC * 20)
    rbias = np.ascontiguousarray(np.stack(rbl, axis=1))
    return params, wr, rbias


_CACHE = {}


def kernel(**inputs):
    ncores = 8
    nseq = 2
    key = "full"
    if key not in _CACHE:
        _CACHE[key] = Builder(nseq=nseq, T=SEQ)
    bld = _CACHE[key]
    params, wr, rbias = host_layout(inputs)
    f = lambda a: np.ascontiguousarray(np.asarray(a, dtype=np.float32))
    x = f(inputs["x"])
    mem = f(inputs["mem"])
    shared = {
        "pool_w": f(inputs["pool_w"])[0], "rg_w_in": f(inputs["rg_w_in"])[0], "rg_w_gate": f(inputs["rg_w_gate"])[0],
        "rg_w_out": f(inputs["rg_w_out"])[0], "xa_wq": f(inputs["xa_wq"]), "xa_wkv": f(inputs["xa_wkv"]),
        "xa_wo": f(inputs["xa_wo"]), "moe_w_gu": f(inputs["moe_w_gu"]), "moe_w_down": f(inputs["moe_w_down"]),
        "params": params, "wr": wr, "rbias": rbias,
    }
    in_maps = []
    for c in range(ncores):
        d = dict(shared)
        d["x"] = x[c * nseq:(c + 1) * nseq]
        d["mem"] = mem[c * nseq:(c + 1) * nseq]
        in_maps.append(d)
    res = run_bass_kernel_spmd(bld.nc, in_maps, core_ids=list(range(ncores)))
    return np.concatenate([np.asarray(r["out"]) for r in res.results], axis=0).astype(np.float32)
```
